# Optimizing a Trainium2 kernel written in Bass

```python
import math
import jax, jax.numpy as jnp
from jax import lax
import numpy as np

D_MODEL = 1024
BATCH = 8
SEQ = 4096
DEPTH = 2

RG_WIDTH = D_MODEL // 2
RG_BLOCKS = 8
RG_BLOCK = RG_WIDTH // RG_BLOCKS
CONV_WIDTH = 4
RG_C = 8.0
GLA_HEADS = 4
GLA_VDIM = D_MODEL // 2
GLA_KDIM = GLA_VDIM // 2
GLA_HK = GLA_KDIM // GLA_HEADS
GLA_HV = GLA_VDIM // GLA_HEADS
GLA_LOWRANK = 16
GLA_GATE_TAU = 16.0
GLA_CHUNK = 64
EVEN_IN = 2 * RG_WIDTH + 2 * GLA_KDIM + 2 * GLA_VDIM + GLA_LOWRANK
MIX_WIDTH = RG_WIDTH + GLA_VDIM

FOX_HEADS = 16
FOX_HD = D_MODEL // FOX_HEADS
FOX_BLOCK = 128
ODD_IN = 4 * D_MODEL + FOX_HEADS

D_FF = 4 * D_MODEL
N_EVEN = (DEPTH + 1) // 2
N_ODD = DEPTH // 2
ALPHA = (2 * DEPTH) ** 0.25
BETA = (8 * DEPTH) ** -0.25
LN_EPS = 1e-5
RMS_EPS = 1e-6

kernel_name = "hybrid_rglru_gla_fox_deepnorm_adaln"


def layer_norm(x, g, b):
    xf = x.astype(jnp.float32)
    mu = jnp.mean(xf, axis=-1, keepdims=True)
    var = jnp.mean(jnp.square(xf - mu), axis=-1, keepdims=True)
    return ((xf - mu) * lax.rsqrt(var + LN_EPS) * g.astype(jnp.float32)
            + b.astype(jnp.float32)).astype(x.dtype)


def rms_norm(x, g):
    xf = x.astype(jnp.float32)
    y = xf * lax.rsqrt(jnp.mean(xf * xf, axis=-1, keepdims=True) + RMS_EPS)
    return (y * g.astype(jnp.float32)).astype(x.dtype)


def ada_mod(c, w, b):
    m = (jax.nn.silu(c) @ w + b)[:, None, :]
    shift, scale, gate = jnp.split(m, 3, axis=-1)
    return shift, scale, 1.0 + gate


def causal_dwconv(x, w, b):
    k_len, ch = w.shape
    y = lax.conv_general_dilated(
        x, w[:, None, :].astype(x.dtype), window_strides=(1,),
        padding=[(k_len - 1, 0)], dimension_numbers=('NWC', 'WIO', 'NWC'),
        feature_group_count=ch)
    return y + b


def rg_lru(x, w_a, b_a, w_x, b_x, lam):
    bn, sn, _ = x.shape
    xf = x.astype(jnp.float32)
    xb = xf.reshape(bn, sn, RG_BLOCKS, RG_BLOCK)
    r = jax.nn.sigmoid(jnp.einsum('bsni,nij->bsnj', xb, w_a.astype(jnp.float32)).reshape(bn, sn, RG_WIDTH) + b_a)
    i = jax.nn.sigmoid(jnp.einsum('bsni,nij->bsnj', xb, w_x.astype(jnp.float32)).reshape(bn, sn, RG_WIDTH) + b_x)
    log_a = RG_C * r * jax.nn.log_sigmoid(lam.astype(jnp.float32))
    a = jnp.exp(log_a)
    u = jnp.sqrt(-jnp.expm1(2.0 * log_a)) * (i * xf)

    def combine(left, right):
        a1, h1 = left
        a2, h2 = right
        return a1 * a2, a2 * h1 + h2

    _, h = lax.associative_scan(combine, (a, u), axis=1)
    return h.astype(x.dtype)


def gla_chunked(q, k, v, log_alpha):
    bn, sn, nh, dk = q.shape
    dv = v.shape[-1]
    n_chunks = sn // GLA_CHUNK

    def blocks(t):
        return t.astype(jnp.float32).reshape(bn, n_chunks, GLA_CHUNK, nh, t.shape[-1]).transpose(1, 0, 3, 2, 4)

    qc = blocks(q) * (dk ** -0.5)
    kc = blocks(k)
    vc = blocks(v)
    bcum = jnp.cumsum(blocks(log_alpha), axis=3)
    b_last = bcum[:, :, :, -1:, :]
    q_dec = qc * jnp.exp(bcum)
    k_dec = kc * jnp.exp(-bcum)
    k_end = kc * jnp.exp(b_last - bcum)
    causal = jnp.tril(jnp.ones((GLA_CHUNK, GLA_CHUNK), dtype=bool))
    att = jnp.where(causal, jnp.einsum('nbhtd,nbhsd->nbhts', q_dec, k_dec), 0.0)
    o_intra = jnp.einsum('nbhts,nbhsv->nbhtv', att, vc)
    kv = jnp.einsum('nbhsd,nbhsv->nbhdv', k_end, vc)
    decay = jnp.exp(b_last[:, :, :, 0, :])

    def step(state, inp):
        dcy, kv_n = inp
        return dcy[..., None] * state + kv_n, state

    _, s_prev = lax.scan(step, jnp.zeros((bn, nh, dk, dv), jnp.float32), (decay, kv))
    o = o_intra + jnp.einsum('nbhtd,nbhdv->nbhtv', q_dec, s_prev)
    return o.transpose(1, 0, 3, 2, 4).reshape(bn, sn, nh, dv).astype(q.dtype)


def even_mixer(u, w_in, conv_w, conv_b, rg_wa, rg_ba, rg_wx, rg_bx, rg_lam,
               gla_w_up, gla_b_up, gla_norm_g, w_out):
    bn, sn, _ = u.shape
    proj = u @ w_in
    s1 = RG_WIDTH
    s2 = s1 + RG_WIDTH
    s3 = s2 + GLA_KDIM
    s4 = s3 + GLA_KDIM
    s5 = s4 + GLA_VDIM
    s6 = s5 + GLA_VDIM
    xr, yr, q, k, v, g, zl = jnp.split(proj, [s1, s2, s3, s4, s5, s6], axis=-1)
    h = rg_lru(causal_dwconv(xr, conv_w, conv_b), rg_wa, rg_ba, rg_wx, rg_bx, rg_lam)
    rg_out = h * jax.nn.gelu(yr)
    log_alpha = jax.nn.log_sigmoid((zl @ gla_w_up + gla_b_up).astype(jnp.float32)) / GLA_GATE_TAU
    o = gla_chunked(q.reshape(bn, sn, GLA_HEADS, GLA_HK),
                    k.reshape(bn, sn, GLA_HEADS, GLA_HK),
                    v.reshape(bn, sn, GLA_HEADS, GLA_HV),
                    log_alpha.reshape(bn, sn, GLA_HEADS, GLA_HK))
    o = rms_norm(o, gla_norm_g.reshape(GLA_HEADS, GLA_HV)) * jax.nn.silu(g).reshape(bn, sn, GLA_HEADS, GLA_HV)
    mix = jnp.concatenate([rg_out, o.reshape(bn, sn, GLA_VDIM)], axis=-1)
    return mix @ w_out


def fox_attention(u, w_in, b_f, q_norm_g, k_norm_g, w_out):
    bn, sn, _ = u.shape
    proj = u @ w_in
    q, k, v, g, fl = jnp.split(proj, [D_MODEL, 2 * D_MODEL, 3 * D_MODEL, 4 * D_MODEL], axis=-1)
    q = rms_norm(q.reshape(bn, sn, FOX_HEADS, FOX_HD), q_norm_g).transpose(0, 2, 1, 3) * (FOX_HD ** -0.5)
    k = rms_norm(k.reshape(bn, sn, FOX_HEADS, FOX_HD), k_norm_g).transpose(0, 2, 1, 3)
    v = v.reshape(bn, sn, FOX_HEADS, FOX_HD).transpose(0, 2, 1, 3)
    log_f = jax.nn.log_sigmoid((fl + b_f).astype(jnp.float32))
    f_cum = jnp.cumsum(log_f, axis=1).transpose(0, 2, 1)
    outs = []
    for blk in range(sn // FOX_BLOCK):
        t0 = blk * FOX_BLOCK
        t1 = t0 + FOX_BLOCK
        s = jnp.einsum('bhtd,bhsd->bhts', q[:, :, t0:t1], k[:, :, :t1]).astype(jnp.float32)
        s = s + f_cum[:, :, t0:t1, None] - f_cum[:, :, None, :t1]
        mask = (t0 + jnp.arange(FOX_BLOCK))[:, None] >= jnp.arange(t1)[None, :]
        p = jax.nn.softmax(jnp.where(mask, s, -jnp.inf), axis=-1)
        outs.append(jnp.einsum('bhts,bhsd->bhtd', p.astype(v.dtype), v[:, :, :t1]))
    o = jnp.concatenate(outs, axis=2).transpose(0, 2, 1, 3).reshape(bn, sn, D_MODEL)
    return (o * jax.nn.sigmoid(g)) @ w_out


def setup_inputs(seed: int = 0) -> dict:
    key = jax.random.key(seed)
    ks = jax.random.split(key, 32)
    D = D_MODEL

    def nrm(i, shape, scale):
        return scale * jax.random.normal(ks[i], shape, jnp.float32)

    lam_a = jax.random.uniform(ks[13], (N_EVEN, RG_WIDTH), jnp.float32, 0.9, 0.999)
    lam_root = lam_a ** (1.0 / RG_C)
    ev_lam = jnp.log(lam_root) - jnp.log1p(-lam_root)
    return {
        "x": nrm(0, (BATCH, SEQ, D), 1.0),
        "c": nrm(1, (BATCH, D), 1.0),
        "ada_w": nrm(2, (DEPTH, 2, D, 3 * D), 0.1 * D ** -0.5),
        "ada_b": nrm(3, (DEPTH, 2, 3 * D), 0.02),
        "ln_g": 1.0 + nrm(4, (DEPTH, 2, D), 0.05),
        "ln_b": nrm(5, (DEPTH, 2, D), 0.02),
        "ev_w_in": nrm(6, (N_EVEN, D, EVEN_IN), D ** -0.5),
        "ev_conv_w": nrm(7, (N_EVEN, CONV_WIDTH, RG_WIDTH), CONV_WIDTH ** -0.5),
        "ev_conv_b": nrm(8, (N_EVEN, RG_WIDTH), 0.02),
        "ev_rg_wa": nrm(9, (N_EVEN, RG_BLOCKS, RG_BLOCK, RG_BLOCK), RG_BLOCK ** -0.5),
        "ev_rg_ba": nrm(10, (N_EVEN, RG_WIDTH), 0.02),
        "ev_rg_wx": nrm(11, (N_EVEN, RG_BLOCKS, RG_BLOCK, RG_BLOCK), RG_BLOCK ** -0.5),
        "ev_rg_bx": nrm(12, (N_EVEN, RG_WIDTH), 0.02),
        "ev_rg_lam": ev_lam,
        "ev_gla_w_up": nrm(14, (N_EVEN, GLA_LOWRANK, GLA_KDIM), GLA_LOWRANK ** -0.5),
        "ev_gla_b_up": nrm(15, (N_EVEN, GLA_KDIM), 0.02),
        "ev_gla_norm_g": 1.0 + nrm(16, (N_EVEN, GLA_VDIM), 0.05),
        "ev_w_out": nrm(17, (N_EVEN, MIX_WIDTH, D), BETA * MIX_WIDTH ** -0.5),
        "od_w_in": nrm(18, (N_ODD, D, ODD_IN), D ** -0.5),
        "od_b_f": 3.0 + 3.0 * jax.random.uniform(ks[19], (N_ODD, FOX_HEADS), jnp.float32),
        "od_q_norm_g": 1.0 + nrm(20, (N_ODD, FOX_HD), 0.05),
        "od_k_norm_g": 1.0 + nrm(21, (N_ODD, FOX_HD), 0.05),
        "od_w_out": nrm(22, (N_ODD, D, D), BETA * D ** -0.5),
        "mlp_w1": nrm(23, (DEPTH, D, D_FF), D ** -0.5),
        "mlp_b1": nrm(24, (DEPTH, D_FF), 0.02),
        "mlp_w2": nrm(25, (DEPTH, D_FF, D), BETA * D_FF ** -0.5),
        "mlp_b2": nrm(26, (DEPTH, D), 0.02),
    }


def reference(x, c, ada_w, ada_b, ln_g, ln_b,
              ev_w_in, ev_conv_w, ev_conv_b, ev_rg_wa, ev_rg_ba, ev_rg_wx, ev_rg_bx, ev_rg_lam,
              ev_gla_w_up, ev_gla_b_up, ev_gla_norm_g, ev_w_out,
              od_w_in, od_b_f, od_q_norm_g, od_k_norm_g, od_w_out,
              mlp_w1, mlp_b1, mlp_w2, mlp_b2):
    for layer in range(DEPTH):
        shift, scale, gate = ada_mod(c, ada_w[layer, 0], ada_b[layer, 0])
        u = x * (1.0 + scale) + shift
        if layer % 2 == 0:
            e = layer // 2
            y = even_mixer(u, ev_w_in[e], ev_conv_w[e], ev_conv_b[e], ev_rg_wa[e], ev_rg_ba[e],
                           ev_rg_wx[e], ev_rg_bx[e], ev_rg_lam[e], ev_gla_w_up[e], ev_gla_b_up[e],
                           ev_gla_norm_g[e], ev_w_out[e])
        else:
            o = layer // 2
            y = fox_attention(u, od_w_in[o], od_b_f[o], od_q_norm_g[o], od_k_norm_g[o], od_w_out[o])
        x = layer_norm(ALPHA * x + gate * y, ln_g[layer, 0], ln_b[layer, 0])
        shift, scale, gate = ada_mod(c, ada_w[layer, 1], ada_b[layer, 1])
        u = x * (1.0 + scale) + shift
        y = jnp.square(jax.nn.relu(u @ mlp_w1[layer] + mlp_b1[layer])) @ mlp_w2[layer] + mlp_b2[layer]
        x = layer_norm(ALPHA * x + gate * y, ln_g[layer, 1], ln_b[layer, 1])
    return x
```

```python
import numpy as np
from contextlib import ExitStack
import concourse.bass as bass
import concourse.mybir as mybir
from concourse.bass_utils import run_bass_kernel_spmd

F32 = mybir.dt.float32
BF16 = mybir.dt.bfloat16
U8 = mybir.dt.uint8
AF = mybir.ActivationFunctionType
ALU = mybir.AluOpType

SEM_CAP = 30000
NTOK = 4096
ALPHA = float((2 * 2) ** 0.25)


class Buf:
    __slots__ = ("name", "w", "r")

    def __init__(self, name):
        self.name = name
        self.w = None
        self.r = []


class V:
    __slots__ = ("buf", "ap")

    def __init__(self, buf, ap):
        self.buf = buf
        self.ap = ap

    def __getitem__(self, idx):
        return V(self.buf, self.ap[idx])

    def rearrange(self, *a, **k):
        return V(self.buf, self.ap.rearrange(*a, **k))

    def bc(self, shape):
        return V(self.buf, self.ap.to_broadcast(list(shape)))


class Sched:
    ENG = ("tensor", "vector", "scalar", "gpsimd", "sync")

    def __init__(self, nc, es):
        self.nc = nc
        self.es = es
        self.ops = {e: [] for e in self.ENG}
        self.same = {"vector", "scalar", "gpsimd"}
        self.dma_pool = {}
        self.dma_rr = {}
        self.n_dma_sems = 24
        self.sems = []
        self.pending = {e: [] for e in self.ENG}
        self.nbuf = 0

    def new_sem(self, name):
        s = self.es.enter_context(self.nc.semaphore(name))
        self.sems.append(s)
        return len(self.sems) - 1

    def view(self, ap, name=None):
        self.nbuf += 1
        return V(Buf(name or f"b{self.nbuf}"), ap)

    def _deps(self, eng, reads, writes):
        deps = []
        for b in reads:
            if b.w is not None:
                deps.append(b.w)
        for b in writes:
            if b.w is not None:
                deps.append(b.w)
            deps.extend(b.r)
        if eng not in self.same:
            deps = [d for d in deps if not (d[0] == "op" and d[1] == eng)]
        if self.pending[eng]:
            deps.extend(self.pending[eng])
            self.pending[eng] = []
        return deps

    def _mark(self, me, reads, writes):
        for b in reads:
            b.r.append(me)
        for b in writes:
            b.w = me
            b.r = []

    def op(self, eng, fn, reads=(), writes=()):
        deps = self._deps(eng, reads, writes)
        me = ("op", eng, len(self.ops[eng]))
        self.ops[eng].append({"fn": fn, "deps": deps, "dma": None})
        self._mark(me, reads, writes)
        return me

    def dma(self, eng, out_ap, in_ap, reads=(), writes=()):
        pool = self.dma_pool.setdefault(eng, [])
        if len(pool) < self.n_dma_sems:
            pool.append([self.new_sem(f"d_{eng}_{len(pool)}"), 0])
            slot = pool[-1]
        else:
            i = self.dma_rr.get(eng, 0)
            slot = pool[i % len(pool)]
            self.dma_rr[eng] = i + 1
        deps = self._deps(eng, reads, writes)
        if slot[1] > 0:
            deps.append(("dma", slot[0], slot[1]))
        slot[1] += 16
        me = ("dma", slot[0], slot[1])
        fn = lambda e, o=out_ap, i=in_ap: e.dma_start(out=o, in_=i)
        self.ops[eng].append({"fn": fn, "deps": deps, "dma": (slot[0], 16)})
        self._mark(me, reads, writes)
        return me

    def ins(self, eng, meth, _r=(), _w=(), **kw):
        reads, writes, real = list(_r), list(_w), {}
        for k, v in kw.items():
            if isinstance(v, V):
                (writes if k in ("out", "accum_out") else reads).append(v.buf)
                real[k] = v.ap
            else:
                real[k] = v
        import sys
        fr = sys._getframe(1)
        while fr.f_code.co_name in ("mm", "act", "tt", "ts", "stt", "cp", "memset") and fr.f_back is not None:
            fr = fr.f_back
        where = f"{fr.f_code.co_name}:{fr.f_lineno}"

        def fn(e, m=meth, r=real, where=where):
            try:
                return getattr(e, m)(**r)
            except BaseException as ex:
                raise RuntimeError(f"emit {m} at {where}: {str(ex)[:600]}") from None
        return self.op(eng, fn, reads, writes)

    def barrier(self):
        tg = []
        for e in self.ENG:
            for idx in range(len(self.ops[e]) - 1, -1, -1):
                if self.ops[e][idx]["dma"] is None and self.ops[e][idx]["fn"] is not None:
                    tg.append(("op", e, idx))
                    break
        for e, pool in self.dma_pool.items():
            for slot in pool:
                if slot[1] > 0:
                    tg.append(("dma", slot[0], slot[1]))
        for e in self.ENG:
            self.pending[e].extend(tg)

    def finish(self):
        self.barrier()
        self.ops["sync"].append({"fn": None, "deps": self._deps("sync", [], []), "dma": None})
        sig = {e: set() for e in self.ENG}
        for e in self.ENG:
            w_op = {}
            w_dma = {}
            for o in self.ops[e]:
                need_op, need_dma = {}, {}
                for d in o["deps"]:
                    if d[0] == "op":
                        if d[1] == e and e not in self.same:
                            continue
                        if need_op.get(d[1], -1) < d[2]:
                            need_op[d[1]] = d[2]
                    else:
                        if need_dma.get(d[1], 0) < d[2]:
                            need_dma[d[1]] = d[2]
                wl = []
                for pe, idx in need_op.items():
                    if w_op.get(pe, -1) < idx:
                        w_op[pe] = idx
                        wl.append(("op", pe, idx))
                        sig[pe].add(idx)
                for s, v in need_dma.items():
                    if w_dma.get(s, 0) < v:
                        w_dma[s] = v
                        wl.append(("dma", s, v))
                o["waits"] = wl
        semval = {}
        for e in self.ENG:
            n = 0
            cur = None
            for idx, o in enumerate(self.ops[e]):
                if idx in sig[e]:
                    if cur is None or n % SEM_CAP == 0:
                        cur = self.new_sem(f"s_{e}_{n // SEM_CAP}")
                    n += 1
                    semval[(e, idx)] = (cur, (n - 1) % SEM_CAP + 1)
        self.n_signal = {e: len(sig[e]) for e in self.ENG}
        nc = self.nc
        sems = self.sems
        with nc.Block() as block:
            def mk(engname):
                def body(e):
                    for idx, o in enumerate(self.ops[engname]):
                        for w in o["waits"]:
                            if w[0] == "op":
                                s, v = semval[(w[1], w[2])]
                            else:
                                s, v = w[1], w[2]
                            e.wait_ge(sems[s], v)
                        if o["fn"] is None:
                            continue
                        i = o["fn"](e)
                        if o["dma"] is not None:
                            i.then_inc(sems[o["dma"][0]], 16)
                        elif (engname, idx) in semval:
                            i.then_inc(sems[semval[(engname, idx)][0]], 1)
                return body
            for engname in self.ENG:
                if self.ops[engname]:
                    getattr(block, engname)(mk(engname))


def build_program(dbg=(), last_phase=5):
    nc = bass.Bass("TRN2", target_bir_lowering=False)

    def inp(name, shape):
        return nc.dram_tensor(name, list(shape), F32, kind="ExternalInput").ap()

    def scratch(name, shape, dt):
        kind = "ExternalOutput" if name in dbg else "Internal"
        return nc.dram_tensor(name, list(shape), dt, kind=kind).ap()

    xT = inp("xT", [1024, NTOK])
    c_fm = inp("c_fm", [128, 8])
    ada_w = inp("ada_w", [4, 128, 8, 3072])
    ada_b = inp("ada_b", [128, 4, 24])
    ln_g = inp("ln_g", [128, 4, 8])
    ln_b = inp("ln_b", [128, 4, 8])
    ev_w_in = inp("ev_w_in", [128, 8, 2576])
    ev_conv_w = inp("ev_conv_w", [128, 4, 4])
    ev_conv_b = inp("ev_conv_b", [128, 4])
    ev_rg_wa = inp("ev_rg_wa", [8, 64, 64])
    ev_rg_ba = inp("ev_rg_ba", [128, 4])
    ev_rg_wx = inp("ev_rg_wx", [8, 64, 64])
    ev_rg_bx = inp("ev_rg_bx", [128, 4])
    ev_rg_lam = inp("ev_rg_lam", [128, 4])
    ev_gla_w_up = inp("ev_gla_w_up", [16, 256])
    ev_gla_b_up = inp("ev_gla_b_up", [64, 4])
    ev_gla_norm_g = inp("ev_gla_norm_g", [128, 4])
    ev_w_out = inp("ev_w_out", [128, 8, 1024])
    od_w_in = inp("od_w_in", [128, 8, 4112])
    od_b_f = inp("od_b_f", [16])
    od_qk_g = inp("od_qk_g", [128, 2])
    od_w_out = inp("od_w_out", [128, 8, 1024])
    mlp_w1 = inp("mlp_w1", [2, 128, 8, 4096])
    mlp_b1 = inp("mlp_b1", [128, 2, 32])
    mlp_w2 = inp("mlp_w2", [2, 128, 32, 1024])
    mlp_b2 = inp("mlp_b2", [128, 2, 8])

    outT = nc.dram_tensor("outT", [1024, NTOK], F32, kind="ExternalOutput").ap()
    x1T = scratch("x1T", [1024, NTOK], F32)
    x2T = scratch("x2T", [1024, NTOK], F32)
    x3T = scratch("x3T", [1024, NTOK], F32)
    qT = scratch("qT", [1024, NTOK], BF16)
    kT = scratch("kT", [1024, NTOK], BF16)
    sgT = scratch("sgT", [1024, NTOK], F32)
    vaug = scratch("vaug", [16, NTOK, 128], BF16)
    w1b = [scratch(f"w1b{l}", [128, 8 * 4096], BF16) for l in range(2)]
    w2b = [scratch(f"w2b{l}", [128, 32 * 1024], BF16) for l in range(2)]
    od_w_in_b = scratch("od_w_in_b", [128, 8 * 4112], BF16)
    od_w_out_b = scratch("od_w_out_b", [128, 8 * 1024], BF16)
    dbg_aps = {}
    for nm, shape in (("d_mod", [128, 96]), ("d_mix", [1024, NTOK]), ("d_oT", [1024, NTOK]), ("d_L", [128, 512])):
        if nm in dbg:
            dbg_aps[nm] = nc.dram_tensor(nm, shape, F32, kind="ExternalOutput").ap()

    with ExitStack() as es:
        S = Sched(nc, es)
        ARENA = 207 * 1024
        arena = es.enter_context(nc.sbuf_tensor("arena", [128, ARENA], U8))
        off = [0]

        def alloc(shape, dt, nparts=128, name=None):
            n = int(np.prod(shape)) * mybir.dt.size(dt)
            n = (n + 31) // 32 * 32
            assert off[0] + n <= ARENA, f"SBUF arena overflow at {name}: {off[0] + n}"
            ap = arena[0:nparts, off[0]:off[0] + n].bitcast(dt)
            if int(np.prod(shape)) * mybir.dt.size(dt) != n:
                ap = ap[:, 0:int(np.prod(shape))]
            off[0] += n
            if len(shape) == 2:
                ap = ap.rearrange("p (a b) -> p a b", b=shape[1])
            elif len(shape) == 3:
                ap = ap.rearrange("p (a b c) -> p a b c", b=shape[1], c=shape[2])
            return S.view(ap, name)

        banks = [S.view(es.enter_context(nc.psum_tensor(f"pb{i}", [128, 512], F32))[:], f"bank{i}") for i in range(8)]
        rr = {"list": list(range(8)), "i": 0}

        def pb():
            b = banks[rr["list"][rr["i"] % len(rr["list"])]]
            rr["i"] += 1
            return b

        def set_banks(lst):
            rr["list"] = list(lst)
            rr["i"] = 0

        def mm(out, lhsT, rhs, start=True, stop=True):
            S.ins("tensor", "matmul", out=out, lhsT=lhsT, rhs=rhs, start=start, stop=stop)

        def act(out, in_, func, bias=None, scale=None):
            kw = {}
            if bias is not None:
                kw["bias"] = bias
            if scale is not None:
                kw["scale"] = scale
            S.ins("scalar", "activation", out=out, in_=in_, func=func, **kw)

        def tt(eng, out, in0, in1, op):
            S.ins(eng, "tensor_tensor", out=out, in0=in0, in1=in1, op=op)

        def ts(eng, out, in0, s1, s2, op0, op1=None):
            if op1 is None:
                S.ins(eng, "tensor_scalar", out=out, in0=in0, scalar1=s1, scalar2=None, op0=op0)
            else:
                S.ins(eng, "tensor_scalar", out=out, in0=in0, scalar1=s1, scalar2=s2, op0=op0, op1=op1)

        def stt(eng, out, in0, scalar, in1, op0, op1):
            S.ins(eng, "scalar_tensor_tensor", out=out, in0=in0, scalar=scalar, in1=in1, op0=op0, op1=op1)

        def cp(eng, out, in_):
            if eng == "scalar":
                act(out, in_, AF.Identity)
            else:
                S.ins(eng, "tensor_copy", out=out, in_=in_)

        def memset(eng, v, val):
            S.ins(eng, "memset", _w=[v.buf], ap=v.ap, constant=val)

        def load(v, src, eng="sync"):
            S.dma(eng, v.ap, src, writes=[v.buf])

        def store(dst, v, eng="sync"):
            S.dma(eng, dst, v.ap, reads=[v.buf])

        class ColBlocks:
            def __init__(self, full, bounds):
                self.bounds = list(bounds)
                self.blk = [V(Buf(f"{full.buf.name}_c{i}"), full.ap[:, :, bounds[i]:bounds[i + 1]])
                            for i in range(len(bounds) - 1)]

            def get(self, kc, c0, c1):
                for i in range(len(self.blk)):
                    if self.bounds[i] <= c0 and c1 <= self.bounds[i + 1]:
                        return self.blk[i][:, kc, c0 - self.bounds[i]:c1 - self.bounds[i]]
                raise AssertionError(f"column range {c0}:{c1} straddles blocks {self.bounds}")

        modv = alloc([4, 24], F32, name="modv")
        gb2 = alloc([2, 8], F32, name="gb2")
        lng = alloc([4, 8], F32, name="lng")
        lnb = alloc([4, 8], F32, name="lnb")
        b1_sb = alloc([2, 32], F32, name="b1")
        b2_sb = alloc([2, 8], F32, name="b2")
        ident = alloc([128], BF16, name="ident")
        onesD = alloc([128], BF16, name="onesD")
        eps_ln = alloc([1], F32, name="eps_ln")
        eps_rms = alloc([1], F32, name="eps_rms")
        L_tm = alloc([32, 16], F32, name="L_tm")
        cumtot = alloc([32, 16], F32, name="cumtot")

        load(lng, ln_g)
        load(lnb, ln_b)
        load(b1_sb, mlp_b1)
        load(b2_sb, mlp_b2)
        memset("gpsimd", ident, 0.0)
        S.ins("gpsimd", "affine_select", out=ident, in_=ident, pattern=[[-1, 128]], compare_op=ALU.not_equal,
              fill=1.0, base=0, channel_multiplier=1)
        memset("gpsimd", onesD, 1.0 / 1024.0)
        memset("gpsimd", eps_ln, 1e-5)
        memset("gpsimd", eps_rms, 1e-6)
        persist_mark = off[0]

        def ln_stages(r, out, li, T, tmp, after=None):
            rb, sq, mean_sb, rstd = tmp
            st = {}

            def T1():
                for kc in range(8):
                    cp("gpsimd", rb[:, kc, :], r[:, kc, :])
                    act(sq[:, kc, :], r[:, kc, :], AF.Square)

            def T2():
                st["m"] = pb()
                st["q"] = pb()
                for kc in range(8):
                    mm(st["m"][:, 0:T], onesD, rb[:, kc, :], start=(kc == 0), stop=(kc == 7))
                for kc in range(8):
                    mm(st["q"][:, 0:T], onesD, sq[:, kc, :], start=(kc == 0), stop=(kc == 7))

            def T3():
                cp("vector", mean_sb, st["m"][:, 0:T])
                tt("vector", rstd, mean_sb, mean_sb, ALU.mult)
                tt("vector", rstd, st["q"][:, 0:T], rstd, ALU.subtract)
                act(rstd, rstd, AF.Ln, bias=eps_ln[:, 0:1])
                act(rstd, rstd, AF.Exp, scale=-0.5)

            def T4a():
                for kc in range(8):
                    tt("vector", out[:, kc, :], r[:, kc, :], mean_sb, ALU.subtract)
                    tt("gpsimd", out[:, kc, :], out[:, kc, :], rstd, ALU.mult)

            def T4b():
                for kc in range(8):
                    act(out[:, kc, :], out[:, kc, :], AF.Identity, bias=lnb[:, li, kc:kc + 1], scale=lng[:, li, kc:kc + 1])
                if after is not None:
                    after()
            return T1, T2, T3, T4a, T4b


        set_banks(range(8))
        p1_w_in = alloc([8, 2576], BF16, name="w_in")
        p1_w_out = alloc([8, 1024], BF16, name="w_out")
        p1_w_inB = ColBlocks(p1_w_in, [0, 512, 1024, 1536, 2048, 2576])
        p1_mark = off[0]
        c_sb = alloc([8], F32, name="c_sb")
        sc_bf = alloc([8], BF16, name="sc_bf")
        adab_sb = alloc([4, 24], F32, name="adab")
        wada = [alloc([8, 768], BF16, name=f"wada{i}") for i in range(2)]
        load(c_sb, c_fm)
        load(adab_sb, ada_b)
        act(sc_bf, c_sb, AF.Silu)
        ps_ada = pb()
        for a in range(4):
            if a == 1:
                for i in range(5):
                    b0, b1 = p1_w_inB.bounds[i], p1_w_inB.bounds[i + 1]
                    load(p1_w_inB.blk[i], ev_w_in[:, :, b0:b1], eng="gpsimd")
                for kc in range(0, 8, 4):
                    load(p1_w_out[:, kc:kc + 4, :], ev_w_out[:, kc:kc + 4, :], eng="gpsimd")
            for q in range(4):
                slot = wada[(a * 4 + q) % 2]
                load(slot, ada_w[a][:, :, q * 768:(q + 1) * 768], eng="gpsimd")
                for oc in range(6):
                    col = a * 24 + q * 6 + oc
                    for kc in range(8):
                        mm(ps_ada[:, col:col + 1], slot[:, kc, oc * 128:(oc + 1) * 128], sc_bf[:, kc:kc + 1],
                           start=(kc == 0), stop=(kc == 7))
        tt("vector", modv, ps_ada[:, 0:96].rearrange("p (a b) -> p a b", b=24), adab_sb, ALU.add)
        ts("vector", modv[:, :, 8:24], modv[:, :, 8:24], 1.0, None, ALU.add)
        for l in range(2):
            tt("vector", gb2[:, l, :], modv[:, 2 * l + 1, 16:24], b2_sb[:, l, :], ALU.mult)
        if "d_mod" in dbg:
            store(dbg_aps["d_mod"], modv.rearrange("p a b -> p (a b)"))
        S.barrier()
        off[0] = p1_mark

        def shift_(a, kc):
            return modv[:, a, kc:kc + 1]

        def scale_(a, kc):
            return modv[:, a, 8 + kc:9 + kc]

        def gate_(a, kc):
            return modv[:, a, 16 + kc:17 + kc]

        def phase1():
            set_banks(range(8))
            GS = 256
            w_out = p1_w_out
            w_inB = p1_w_inB
            convw = alloc([4, 4], F32, name="convw")
            convb = alloc([4], F32, name="convb")
            ba = alloc([4], F32, name="ba")
            bx = alloc([4], F32, name="bx")
            lam = alloc([4], F32, name="lam")
            cl = alloc([4], F32, name="cl")
            cl2 = alloc([4], F32, name="cl2")
            nbup = alloc([4], F32, nparts=64, name="nbup")
            gng = alloc([4], F32, name="gng")
            load(convw, ev_conv_w)
            load(convb, ev_conv_b)
            load(ba, ev_rg_ba)
            load(bx, ev_rg_bx)
            load(lam, ev_rg_lam)
            load(nbup, ev_gla_b_up)
            load(gng, ev_gla_norm_g)
            ts("vector", nbup, nbup, -1.0, None, ALU.mult)
            act(cl, lam, AF.Exp, scale=-1.0)
            act(cl, cl, AF.Ln, bias=1.0)
            ts("vector", cl2, cl, -16.0, None, ALU.mult)
            ts("vector", cl, cl, -8.0, None, ALU.mult)
            wa_f = alloc([4, 128], F32, name="wa_f")
            wx_f = alloc([4, 128], F32, name="wx_f")
            wa_bd = alloc([4, 128], BF16, name="wa_bd")
            wx_bd = alloc([4, 128], BF16, name="wx_bd")
            memset("gpsimd", wa_f, 0.0)
            memset("gpsimd", wx_f, 0.0)
            for cc in range(4):
                for e in range(2):
                    S.dma("sync", wa_f.ap[e * 64:(e + 1) * 64, cc, e * 64:(e + 1) * 64], ev_rg_wa[2 * cc + e], writes=[wa_f.buf])
                    S.dma("sync", wx_f.ap[e * 64:(e + 1) * 64, cc, e * 64:(e + 1) * 64], ev_rg_wx[2 * cc + e], writes=[wx_f.buf])
            cp("vector", wa_bd, wa_f)
            cp("vector", wx_bd, wx_f)
            wup_f = alloc([256], F32, nparts=16, name="wup_f")
            wup_bf = alloc([256], BF16, nparts=16, name="wup_bf")
            load(wup_f, ev_gla_w_up)
            cp("vector", wup_bf, wup_f)
            ones128 = alloc([128], BF16, name="ones128")
            memset("gpsimd", ones128, 1.0 / 128.0)
            mask4 = alloc([4, 64], F32, nparts=64, name="mask4")
            memset("gpsimd", mask4, 1.0)
            S.ins("gpsimd", "affine_select", out=mask4, in_=mask4, pattern=[[0, 4], [1, 64]], compare_op=ALU.is_ge,
                  fill=0.0, base=0, channel_multiplier=-1)
            cmask = alloc([4 * GS // 64, 64], F32, nparts=64, name="cmask")
            memset("gpsimd", cmask, 1.0)
            memset("gpsimd", cmask[:, :, 0:1], 0.0)
            Sst = alloc([4, 128], F32, nparts=64, name="Sst")
            Sbf = alloc([4, 128], BF16, nparts=64, name="Sbf")
            memset("gpsimd", Sst, 0.0)
            memset("gpsimd", Sbf, 0.0)
            hlast = alloc([4], F32, name="hlast")
            memset("gpsimd", hlast, 0.0)
            xg = [alloc([8, GS], F32, name=f"xg{i}") for i in range(2)]
            ug = [alloc([8, GS], BF16, name=f"ug{i}") for i in range(2)]
            xr = [[alloc([GS + 3], F32, name=f"xr{i}_{cc}") for cc in range(4)] for i in range(2)]
            for cc in range(4):
                memset("gpsimd", xr[1][cc][:, GS:GS + 3], 0.0)
            mix = alloc([8, GS], BF16, name="mix")
            rbuf = [alloc([8, GS], F32, name=f"rbuf{i}") for i in range(2)]
            lt = (alloc([8, GS], BF16, name="ln_rb"), alloc([8, GS], BF16, name="ln_sq"),
                  alloc([GS], F32, name="ln_mean"), alloc([GS], F32, name="ln_rstd"))
            tR = [{k: alloc([GS], F32, name=f"t{k}{cc}") for k in "abcde"} for cc in range(4)]
            xcb = [alloc([GS], BF16, name=f"xcb{cc}") for cc in range(4)]
            tO = [alloc([GS], F32, name=f"tO{i}") for i in range(3)]
            q_sb = alloc([4, GS], F32, nparts=64, name="q_sb")
            k_sb = alloc([4, GS], F32, nparts=64, name="k_sb")
            l_sb = alloc([4, GS], F32, nparts=64, name="l_sb")
            cs_sb = alloc([4, GS], F32, nparts=64, name="cs_sb")
            eb = alloc([4, GS], F32, nparts=64, name="eb")
            enb = alloc([4, GS], F32, nparts=64, name="enb")
            qd = alloc([4, GS], BF16, nparts=64, name="qd")
            kd = alloc([4, GS], BF16, nparts=64, name="kd")
            zl_bf = alloc([GS], BF16, nparts=16, name="zl_bf")
            v_n = [alloc([512], BF16, nparts=64, name=f"v_n{i}") for i in range(2)]
            kd_n = [alloc([256], BF16, nparts=64, name=f"kd_n{i}") for i in range(2)]
            att_sb = [alloc([4, 64], BF16, nparts=64, name=f"att{i}") for i in range(2)]
            o_g = alloc([4, GS], F32, name="o_g")
            osq = [alloc([GS], BF16, name=f"osq{h}") for h in range(4)]
            tmpS = alloc([4, 128], F32, nparts=64, name="tmpS")

            NG = NTOK // GS
            NCH = GS // 64
            xv = xT.rearrange("(kc p) t -> p kc t", p=128)
            def bg_cast(dst, src2d, npieces):
                n = src2d.shape[1]
                step = n // npieces
                for i in range(npieces):
                    S.dma("gpsimd", dst[:, i * step:(i + 1) * step], src2d[:, i * step:(i + 1) * step])

            def rg_front(g):
                U_ = ug[g % 2]
                XR_ = [xr[g % 2][cc] for cc in range(4)]
                XRp_ = [xr[(g + 1) % 2][cc] for cc in range(4)]
                for cc in range(4):
                    ps = pb()[:, 0:GS]
                    for kc in range(8):
                        mm(ps, w_inB.get(kc, cc * 128, (cc + 1) * 128), U_[:, kc, :], start=(kc == 0), stop=(kc == 7))
                    cp("scalar", XR_[cc][:, 3:GS + 3], ps)
                    cp("gpsimd", XR_[cc][:, 0:3], XRp_[cc][:, GS:GS + 3])
                for cc in range(4):
                    ps2 = pb()[:, 0:GS]
                    for kc in range(8):
                        mm(ps2, w_inB.get(kc, 512 + cc * 128, 512 + (cc + 1) * 128), U_[:, kc, :], start=(kc == 0), stop=(kc == 7))
                    act(tR[cc]["e"], ps2, AF.Gelu_apprx_tanh)

            def p1_make_u(g):
                for kc in range(8):
                    act(ug[g % 2][:, kc, :], xg[g % 2][:, kc, :], AF.Identity, bias=shift_(0, kc), scale=scale_(0, kc))

            load(xg[0], xv[:, :, 0:GS])
            prev = None
            oi = 0
            for g in range(NG):
                t0 = g * GS
                X = xg[g % 2]
                U = ug[g % 2]
                if g + 1 < NG:
                    load(xg[(g + 1) % 2], xv[:, :, t0 + GS:t0 + 2 * GS])
                if g == 0:
                    p1_make_u(0)
                if g == 1:
                    bg_cast(w1b[0], mlp_w1[0].rearrange("p k n -> p (k n)"), 4)
                    bg_cast(w2b[0], mlp_w2[0].rearrange("p k n -> p (k n)"), 4)
                if g == 5:
                    bg_cast(od_w_in_b, od_w_in.rearrange("p k n -> p (k n)"), 4)
                    bg_cast(od_w_out_b, od_w_out.rearrange("p k n -> p (k n)"), 1)
                if g == 8:
                    bg_cast(w1b[1], mlp_w1[1].rearrange("p k n -> p (k n)"), 4)
                    bg_cast(w2b[1], mlp_w2[1].rearrange("p k n -> p (k n)"), 4)
                XR = [xr[g % 2][cc] for cc in range(4)]
                if g == 0:
                    rg_front(0)
                if prev is not None:
                    prev[1]()
                    prev[2]()
                    prev[3]()
                for cc in range(4):
                    ts("vector", tR[cc]["a"], XR[cc][:, 3:GS + 3], convw[:, cc, 3:4], convb[:, cc:cc + 1], ALU.mult, ALU.add)
                for tap in (2, 1, 0):
                    for cc in range(4):
                        stt("vector", tR[cc]["a"], XR[cc][:, tap:GS + tap], convw[:, cc, tap:tap + 1], tR[cc]["a"], ALU.mult, ALU.add)
                for cc in range(4):
                    cp("gpsimd", xcb[cc], tR[cc]["a"])
                psr, psi = [], []
                for cc in range(4):
                    p1 = pb()[:, 0:GS]
                    mm(p1, wa_bd[:, cc, :], xcb[cc])
                    p2 = pb()[:, 0:GS]
                    mm(p2, wx_bd[:, cc, :], xcb[cc])
                    psr.append(p1)
                    psi.append(p2)
                for cc in range(4):
                    act(tR[cc]["b"], psr[cc], AF.Sigmoid, bias=ba[:, cc:cc + 1])
                    act(tR[cc]["c"], psi[cc], AF.Sigmoid, bias=bx[:, cc:cc + 1])
                for cc in range(4):
                    act(tR[cc]["d"], tR[cc]["b"], AF.Exp, scale=cl2[:, cc:cc + 1])
                    act(tR[cc]["b"], tR[cc]["b"], AF.Exp, scale=cl[:, cc:cc + 1])
                for cc in range(4):
                    act(tR[cc]["d"], tR[cc]["d"], AF.Sqrt, bias=1.0, scale=-1.0)
                    tt("gpsimd", tR[cc]["c"], tR[cc]["c"], tR[cc]["a"], ALU.mult)
                for h in range(4):
                    ps = pb()[:, 0:GS]
                    for kc in range(8):
                        mm(ps[0:64, :], w_inB.get(kc, 1024 + h * 64, 1024 + (h + 1) * 64), U[:, kc, :], start=(kc == 0), stop=(kc == 7))
                    cp("scalar", q_sb[:, h, :], ps[0:64, :])
                    ps = pb()[:, 0:GS]
                    for kc in range(8):
                        mm(ps[0:64, :], w_inB.get(kc, 1280 + h * 64, 1280 + (h + 1) * 64), U[:, kc, :], start=(kc == 0), stop=(kc == 7))
                    cp("scalar", k_sb[:, h, :], ps[0:64, :])
                ps = pb()[:, 0:GS]
                for kc in range(8):
                    mm(ps[0:16, :], w_inB.get(kc, 2560, 2576), U[:, kc, :], start=(kc == 0), stop=(kc == 7))
                cp("scalar", zl_bf, ps[0:16, :])
                for cc in range(4):
                    tt("vector", tR[cc]["c"], tR[cc]["c"], tR[cc]["d"], ALU.mult)
                for cc in range(4):
                    S.ins("vector", "tensor_tensor_scan", out=tR[cc]["d"], data0=tR[cc]["b"], data1=tR[cc]["c"],
                          initial=hlast[:, cc:cc + 1], op0=ALU.mult, op1=ALU.add)
                for cc in range(4):
                    cp("vector", hlast[:, cc:cc + 1], tR[cc]["d"][:, GS - 1:GS])
                    tt("gpsimd", mix[:, cc, :], tR[cc]["d"], tR[cc]["e"], ALU.mult)
                if prev is not None:
                    prev[4]()
                for h in range(4):
                    ps = pb()[:, 0:GS]
                    mm(ps[0:64, :], wup_bf[:, h * 64:(h + 1) * 64], zl_bf)
                    act(l_sb[:, h, :], ps[0:64, :], AF.Exp, bias=nbup[:, h:h + 1], scale=-1.0)
                act(l_sb, l_sb, AF.Ln, bias=1.0)
                S.ins("vector", "tensor_tensor_scan", out=cs_sb.rearrange("p h t -> p (h t)"),
                      data0=cmask.rearrange("p a b -> p (a b)"), data1=l_sb.rearrange("p h t -> p (h t)"),
                      initial=0.0, op0=ALU.mult, op1=ALU.add)
                act(eb, cs_sb, AF.Exp, scale=-1.0 / 16.0)
                act(enb, cs_sb, AF.Exp, scale=1.0 / 16.0)
                stt("vector", qd, q_sb, 0.125, eb, ALU.mult, ALU.mult)
                tt("gpsimd", kd, k_sb, enb, ALU.mult)
                sgate = []
                for h in range(4):
                    psg = pb()[:, 0:GS]
                    for kc in range(8):
                        mm(psg, w_inB.get(kc, 2048 + h * 128, 2048 + (h + 1) * 128), U[:, kc, :], start=(kc == 0), stop=(kc == 7))
                    act(tR[h]["b"], psg, AF.Silu)
                    sgate.append(tR[h]["b"])

                def chunk_front(n):
                    tc = n * 64
                    VN, KN, AT = v_n[n % 2], kd_n[n % 2], att_sb[n % 2]
                    psv = pb()
                    for kc in range(8):
                        mm(psv[0:64, :], U[:, kc, tc:tc + 64], w_inB.get(kc, 1536, 2048), start=(kc == 0), stop=(kc == 7))
                    cp("scalar", VN, psv[0:64, :])
                    pst = pb()
                    pst_bf = V(pst.buf, pst.ap.bitcast(BF16))
                    for h in range(4):
                        S.ins("tensor", "transpose", out=pst_bf[0:64, h * 64:(h + 1) * 64], in_=kd[:, h, tc:tc + 64],
                              identity=ident[0:64, 0:64])
                    cp("vector", KN, pst_bf[0:64, 0:256])
                    psa = pb()
                    for h in range(4):
                        mm(psa[0:64, h * 64:(h + 1) * 64], kd[:, h, tc:tc + 64], qd[:, h, tc:tc + 64])
                    tt("vector", AT, psa[0:64, 0:256].rearrange("p (h t) -> p h t", h=4), mask4, ALU.mult)

                def chunk_back(n):
                    tc = n * 64
                    VN, KN, AT = v_n[n % 2], kd_n[n % 2], att_sb[n % 2]
                    pso = pb()
                    for h in range(4):
                        mm(pso[:, h * 64:(h + 1) * 64], VN[:, h * 128:(h + 1) * 128], AT[:, h, :], start=True, stop=False)
                        mm(pso[:, h * 64:(h + 1) * 64], Sbf[:, h, :], qd[:, h, tc:tc + 64], start=False, stop=True)
                    cp("scalar", o_g[:, :, tc:tc + 64], pso[:, 0:256].rearrange("p (h t) -> p h t", h=4))
                    psk = pb()
                    for h in range(4):
                        mm(psk[0:64, h * 128:(h + 1) * 128], KN[:, h * 64:(h + 1) * 64], VN[:, h * 128:(h + 1) * 128])
                    tt("vector", tmpS, Sst, psk[0:64, :].rearrange("p (h d) -> p h d", h=4), ALU.add)
                    dec = eb[:, :, tc + 63:tc + 64].bc([64, 4, 128])
                    tt("vector", Sst, tmpS, dec, ALU.mult)
                    cp("gpsimd", Sbf, Sst)

                if g + 1 < NG:
                    p1_make_u(g + 1)
                chunk_front(0)
                for n in range(NCH):
                    if n + 1 < NCH:
                        chunk_front(n + 1)
                    chunk_back(n)
                if g + 1 < NG:
                    rg_front(g + 1)
                psm = []
                for h in range(4):
                    tt("gpsimd", osq[h], o_g[:, h, :], o_g[:, h, :], ALU.mult)
                for h in range(4):
                    p = pb()[:, 0:GS]
                    mm(p, ones128, osq[h])
                    psm.append(p)
                for h in range(4):
                    act(tR[h]["a"], psm[h], AF.Ln, bias=eps_rms[:, 0:1])
                for h in range(4):
                    act(tR[h]["a"], tR[h]["a"], AF.Exp, scale=-0.5)
                for h in range(4):
                    stt("vector", tR[h]["c"], o_g[:, h, :], gng[:, h:h + 1], tR[h]["a"], ALU.mult, ALU.mult)
                for h in range(4):
                    tt("gpsimd", mix[:, 4 + h, :], tR[h]["c"], sgate[h], ALU.mult)
                if "d_mix" in dbg:
                    tmpf = rbuf[(g + 1) % 2]
                    cp("vector", tmpf, mix)
                    store(dbg_aps["d_mix"].rearrange("(kc p) t -> p kc t", p=128)[:, :, t0:t0 + GS], tmpf)
                R = rbuf[g % 2]
                for oc in range(8):
                    a_ = tO[oi % 3]
                    oi += 1
                    ps = pb()[:, 0:GS]
                    for kc in range(8):
                        mm(ps, w_out[:, kc, oc * 128:(oc + 1) * 128], mix[:, kc, :], start=(kc == 0), stop=(kc == 7))
                    act(a_, ps, AF.Identity, scale=gate_(0, oc))
                    stt("vector", R[:, oc, :], X[:, oc, :], ALPHA, a_, ALU.mult, ALU.add)
                stg = ln_stages(R, R, 0, GS, lt, after=(lambda R=R, t0=t0: store(x1T.rearrange("(kc p) t -> p kc t", p=128)[:, :, t0:t0 + GS], R)))
                stg[0]()
                prev = stg
            prev[1]()
            prev[2]()
            prev[3]()
            prev[4]()

        def phase_mlp(l, src, dst):
            set_banks(range(8))
            GS = 256
            a_idx = 2 * l + 1
            li = 2 * l + 1
            w1 = alloc([8, 4096], BF16, name="w1")
            w2 = alloc([32, 1024], BF16, name="w2")
            ug = [alloc([8, GS], BF16, name=f"m_ug{i}") for i in range(2)]
            hT_all = alloc([32, GS], BF16, name="m_hT")
            hT = [V(Buf(f"m_hT{fc}"), hT_all.ap[:, fc, :]) for fc in range(32)]
            rbuf = [alloc([8, GS], F32, name=f"m_r{i}") for i in range(3)]
            lt = (alloc([8, GS], BF16, name="m_rb"), alloc([8, GS], BF16, name="m_sq"),
                  alloc([GS], F32, name="m_mean"), alloc([GS], F32, name="m_rstd"))
            NT = 12
            tA = [alloc([GS], F32, name=f"m_tA{i}") for i in range(NT)]
            srcv = src.rearrange("(kc p) t -> p kc t", p=128)
            dstv = dst.rearrange("(kc p) t -> p kc t", p=128)
            NG = NTOK // GS
            load(rbuf[0], srcv[:, :, 0:GS])
            w1v = w1b[l].rearrange("p (k n) -> p k n", k=8)
            w2v = w2b[l].rearrange("p (k n) -> p k n", k=32)
            w1B = ColBlocks(w1, [j * 512 for j in range(9)])
            w2P = [V(Buf(f"w2_r{j}"), w2.ap[:, j * 4:(j + 1) * 4, :]) for j in range(8)]
            for j in range(8):
                load(w1B.blk[j], w1v[:, :, j * 512:(j + 1) * 512])
            for j in range(8):
                load(w2P[j], w2v[:, j * 4:(j + 1) * 4, :])
            ui = [0]
            prev = None

            def fc_block(U, lo, hi):
                for fc in range(lo, hi):
                    a_ = tA[ui[0] % NT]
                    ui[0] += 1
                    ps = pb()
                    for kc in range(8):
                        mm(ps[:, 0:GS], w1B.get(kc, fc * 128, (fc + 1) * 128), U[:, kc, :], start=(kc == 0), stop=(kc == 7))
                    act(a_, ps[:, 0:GS], AF.Relu, bias=b1_sb[:, l, fc:fc + 1])
                    tt("vector" if fc % 2 == 0 else "gpsimd", hT[fc], a_, a_, ALU.mult)

            def make_u(g):
                for kc in range(8):
                    act(ug[g % 2][:, kc, :], rbuf[g % 3][:, kc, :], AF.Identity, bias=shift_(a_idx, kc), scale=scale_(a_idx, kc))

            make_u(0)
            if NG > 1:
                load(rbuf[1], srcv[:, :, GS:2 * GS])
            for g in range(NG):
                t0 = g * GS
                R = rbuf[g % 3]
                U = ug[g % 2]
                fc_block(U, 0, 8)
                if prev is not None:
                    prev[1]()
                    prev[2]()
                fc_block(U, 8, 14)
                if prev is not None:
                    prev[3]()
                fc_block(U, 14, 20)
                if prev is not None:
                    prev[4]()
                if g + 2 < NG:
                    load(rbuf[(g + 2) % 3], srcv[:, :, t0 + 2 * GS:t0 + 3 * GS])
                fc_block(U, 20, 26)
                if g + 1 < NG:
                    make_u(g + 1)
                fc_block(U, 26, 32)
                for oc in range(8):
                    a_ = tA[ui[0] % NT]
                    ui[0] += 1
                    ps = pb()
                    for fc in range(32):
                        mm(ps[:, 0:GS], w2P[fc // 4][:, fc % 4, oc * 128:(oc + 1) * 128], hT[fc], start=(fc == 0), stop=(fc == 31))
                    act(a_, ps[:, 0:GS], AF.Identity, bias=gb2[:, l, oc:oc + 1], scale=gate_(a_idx, oc))
                    stt("vector", R[:, oc, :], R[:, oc, :], ALPHA, a_, ALU.mult, ALU.add)
                stg = ln_stages(R, R, li, GS, lt, after=(lambda R=R, t0=t0: store(dstv[:, :, t0:t0 + GS], R)))
                stg[0]()
                prev = stg
            prev[1]()
            prev[2]()
            prev[3]()
            prev[4]()

        def phase3():
            set_banks(range(7))
            flb = banks[7]
            GS = 512
            w_in = alloc([8, 4112], BF16, name="o_w_in")
            owv = od_w_in_b.rearrange("p (k n) -> p k n", k=8)
            w_inB = ColBlocks(w_in, [0, 512, 1024, 1536, 2048, 2560, 3072, 3584, 4112])
            for i in range(8):
                b0, b1 = w_inB.bounds[i], w_inB.bounds[i + 1]
                load(w_inB.blk[i], owv[:, :, b0:b1])
            bd64 = alloc([128], BF16, name="bd64")
            memset("gpsimd", bd64, 0.0)
            memset("gpsimd", bd64[0:64, 0:64], 1.0 / 64.0)
            memset("gpsimd", bd64[64:128, 64:128], 1.0 / 64.0)
            qkg = alloc([2], F32, name="qkg")
            load(qkg, od_qk_g)
            ts("vector", qkg[:, 0:1], qkg[:, 0:1], 0.125, None, ALU.mult)
            bf_bc = alloc([16], F32, name="bf_bc")
            load(bf_bc, od_b_f.partition_broadcast(128))
            triU = alloc([128], F32, name="triU")
            memset("gpsimd", triU, 1.0)
            S.ins("gpsimd", "affine_select", out=triU, in_=triU, pattern=[[1, 128]], compare_op=ALU.is_ge,
                  fill=0.0, base=0, channel_multiplier=-1)
            onesF = alloc([128], F32, name="onesF")
            memset("gpsimd", onesF, 1.0)
            ones32 = alloc([32], F32, name="ones32")
            memset("gpsimd", ones32, 1.0)
            xg = [alloc([8, GS], F32, name=f"p3_xg{i}") for i in range(2)]
            ug = [alloc([8, GS], BF16, name=f"p3_ug{i}") for i in range(2)]
            NT = 4
            tA = [alloc([GS], F32, name=f"p3_tA{i}") for i in range(NT)]
            tB = [alloc([GS], F32, name=f"p3_tB{i}") for i in range(NT)]
            sqb = [alloc([GS], BF16, name=f"p3_sq{i}") for i in range(NT)]
            qkout = [alloc([8, GS], BF16, name=f"p3_qk{i}") for i in range(2)]
            sgout = [alloc([GS], F32, name=f"p3_sg{i}") for i in range(3)]
            vst = [alloc([16, 128], BF16, name=f"p3_vst{i}") for i in range(2)]
            for i in range(2):
                memset("gpsimd", vst[i], 1.0)
            srcv = x2T.rearrange("(kc p) t -> p kc t", p=128)
            load(xg[0], srcv[:, :, 0:GS])
            ui = 0
            si = 0
            for g in range(NTOK // GS):
                t0 = g * GS
                X = xg[g % 2]
                U = ug[g % 2]
                if g + 1 < NTOK // GS:
                    load(xg[(g + 1) % 2], srcv[:, :, t0 + GS:t0 + 2 * GS])
                for kc in range(8):
                    act(U[:, kc, :], X[:, kc, :], AF.Identity, bias=shift_(2, kc), scale=scale_(2, kc))
                def qk_A(k):
                    which, c = divmod(k, 8)
                    ps = pb()
                    col = which * 1024 + c * 128
                    for kc in range(8):
                        mm(ps, w_inB.get(kc, col, col + 128), U[:, kc, :], start=(kc == 0), stop=(kc == 7))
                    a_, s_ = tA[k % NT], sqb[k % NT]
                    cp("scalar", a_, ps)
                    tt("gpsimd", s_, a_, a_, ALU.mult)

                def qk_C(k):
                    which, c = divmod(k, 8)
                    a_, b_, s_ = tA[k % NT], tB[k % NT], sqb[k % NT]
                    psm = pb()
                    mm(psm, bd64, s_)
                    act(b_, psm, AF.Ln, bias=eps_rms[:, 0:1])
                    act(b_, b_, AF.Exp, scale=-0.5)
                    stt("vector", qkout[which][:, c, :], a_, qkg[:, which:which + 1], b_, ALU.mult, ALU.mult)
                    if c == 7:
                        store((qT if which == 0 else kT).rearrange("(c p) t -> p c t", p=128)[:, :, t0:t0 + GS], qkout[which])

                for step in range(16 + 2):
                    if step < 16:
                        qk_A(step)
                    if step >= 2:
                        qk_C(step - 2)
                for c in range(8):
                    sg_ = sgout[si % 3]
                    si += 1
                    ps = pb()
                    col = 3072 + c * 128
                    for kc in range(8):
                        mm(ps, w_inB.get(kc, col, col + 128), U[:, kc, :], start=(kc == 0), stop=(kc == 7))
                    act(sg_, ps, AF.Sigmoid)
                    store(sgT[c * 128:(c + 1) * 128, t0:t0 + GS], sg_)
                for tt_ in range(4):
                    j = g * 4 + tt_
                    VS = vst[j % 2]
                    VS4 = VS.rearrange("p (c e) d -> p c e d", e=2)
                    for half in range(2):
                        ps = pb()
                        col = 2048 + half * 512
                        for kc in range(8):
                            mm(ps, U[:, kc, tt_ * 128:(tt_ + 1) * 128], w_inB.get(kc, col, col + 512), start=(kc == 0), stop=(kc == 7))
                        psv = ps.rearrange("p (c e d) -> p c e d", c=4, e=2)
                        cp("scalar", VS4[:, half * 4:(half + 1) * 4, 0, 0:64], psv[:, :, 0, :])
                        cp("vector", VS4[:, half * 4:(half + 1) * 4, 1, 64:128], psv[:, :, 1, :])
                    store(vaug.rearrange("h t d -> t h d")[j * 128:(j + 1) * 128, :, :], VS)
                    for kc in range(8):
                        mm(flb[:, j * 16:(j + 1) * 16], U[:, kc, tt_ * 128:(tt_ + 1) * 128], w_inB.get(kc, 4096, 4112),
                           start=(kc == 0), stop=(kc == 7))
            z = alloc([32, 16], F32, name="p3_z")
            tot = alloc([32, 16], F32, name="p3_tot")
            tt("vector", z, flb.rearrange("p (j h) -> p j h", h=16), bf_bc.rearrange("p (o h) -> p o h", o=1).bc([128, 32, 16]), ALU.add)
            act(z, z, AF.Exp, scale=-1.0)
            act(z, z, AF.Ln, bias=1.0)
            zf = z.rearrange("p j h -> p (j h)")
            ps_cs = pb()
            mm(ps_cs, triU, zf)
            ps_tot = pb()
            mm(ps_tot, onesF, zf)
            cp("scalar", tot, ps_tot.rearrange("p (j h) -> p j h", h=16))
            for h in range(16):
                S.ins("vector", "tensor_tensor_scan", out=cumtot[:, :, h], data0=ones32, data1=tot[:, :, h], initial=0.0,
                      op0=ALU.mult, op1=ALU.add)
            tt("vector", tot, cumtot, tot, ALU.subtract)
            tt("vector", L_tm, ps_cs.rearrange("p (j h) -> p j h", h=16), tot, ALU.add)
            if "d_L" in dbg:
                store(dbg_aps["d_L"], L_tm.rearrange("p j h -> p (j h)"))

        def phase45():
            GS = 512
            oT = alloc([8, NTOK], BF16, name="oT")
            swp = alloc([128], F32, name="swp")
            memset("gpsimd", swp, 0.0)
            S.ins("gpsimd", "affine_select", out=swp[:, 0:64], in_=swp[:, 0:64], pattern=[[-1, 64]], compare_op=ALU.not_equal,
                  fill=1.0, base=-64, channel_multiplier=1)
            S.ins("gpsimd", "affine_select", out=swp[:, 64:128], in_=swp[:, 64:128], pattern=[[-1, 64]], compare_op=ALU.not_equal,
                  fill=1.0, base=0, channel_multiplier=1)
            negmask = alloc([128], BF16, name="negmask")
            memset("gpsimd", negmask, -30000.0)
            S.ins("gpsimd", "affine_select", out=negmask, in_=negmask, pattern=[[-1, 128]], compare_op=ALU.is_gt,
                  fill=0.0, base=0, channel_multiplier=1)
            p4_mark = off[0]
            qc = [[alloc([NTOK], BF16, name=f"a_q{i}_{e}") for e in range(2)] for i in range(2)]
            for i in range(2):
                memset("gpsimd", qc[i][0][64:128, :], 0.0)
                memset("gpsimd", qc[i][1][0:64, :], 0.0)
            kc_ = [alloc([NTOK], BF16, name=f"a_k{i}") for i in range(2)]
            va = [alloc([32, 2, 128], BF16, name=f"a_v{i}") for i in range(2)]
            biasG = [alloc([32, 2], F32, name=f"a_bias{i}") for i in range(2)]
            NP = 4
            pT = [alloc([GS], BF16, name=f"a_pT{i}") for i in range(NP)]
            sgl = [alloc([GS], F32, name=f"a_sg{i}") for i in range(2)]
            Rr = [alloc([GS], F32, name=f"a_R{i}") for i in range(2)]
            Rg = [alloc([GS], F32, name=f"a_Rg{i}") for i in range(2)]
            stb = [banks[4], banks[5], banks[6]]
            NG = NTOK // GS
            items = []
            for c in range(8):
                for G in range(NG):
                    nj = 4 * G + 4
                    for e in range(2):
                        for j in range(nj):
                            items.append((c, G, e, j, nj))
            n_items = len(items)
            D = 2
            deferred = {}

            def load_pair(c):
                load(qc[c % 2][0][0:64, :], qT[c * 128:c * 128 + 64, :])
                load(qc[c % 2][1][64:128, :], qT[c * 128 + 64:(c + 1) * 128, :])
                load(kc_[c % 2], kT[c * 128:(c + 1) * 128, :])
                for e in range(2):
                    load(va[c % 2][:, :, e, :], vaug[2 * c + e].rearrange("(j p) d -> p j d", p=128))

            def grp(c, G):
                return c * NG + G

            def emitA(k):
                c, G, e, j, nj = items[k]
                it = grp(c, G)
                if e == 0 and j == 0:
                    BG = biasG[it % 2]
                    load(sgl[it % 2], sgT[c * 128:(c + 1) * 128, G * GS:(G + 1) * GS])
                    for e2 in range(2):
                        h = 2 * c + e2
                        ts("vector", BG[:, 0:nj, e2], L_tm[:, 0:nj, h], 1.0, cumtot[:, nj - 1, h:h + 1], ALU.mult, ALU.subtract)
                i = j - 4 * G
                qs = max(0, i) * 128
                N = GS - qs
                st = stb[k % 3]
                if i >= 0:
                    mm(st[:, 0:N], kc_[c % 2][:, j * 128:(j + 1) * 128], qc[c % 2][e][:, G * GS + qs:(G + 1) * GS],
                       start=True, stop=False)
                    mm(st[:, 0:128], ident, negmask, start=False, stop=True)
                else:
                    mm(st[:, 0:N], kc_[c % 2][:, j * 128:(j + 1) * 128], qc[c % 2][e][:, G * GS + qs:(G + 1) * GS])

            def emitBC(k, step):
                c, G, e, j, nj = items[k]
                it = grp(c, G)
                BG = biasG[it % 2]
                O = [banks[(it % 2) * 2], banks[(it % 2) * 2 + 1]]
                i = j - 4 * G
                qs = max(0, i) * 128
                N = GS - qs
                st = stb[k % 3]
                P_ = pT[k % NP]
                act(P_[:, 0:N], st[:, 0:N], AF.Exp, bias=BG[:, j, e:e + 1])
                mm(O[e][:, qs:GS], va[c % 2][:, j, e, :], P_[:, 0:N], start=(j == 0), stop=(j == nj - 1))
                if e == 1 and j == nj - 1:
                    R_ = Rr[it % 2]
                    RG_ = Rg[it % 2]
                    SGL = sgl[it % 2]

                    def fin(c=c, G=G, O=O, R_=R_, RG_=RG_, SGL=SGL):
                        act(R_[0:64, :], O[1][0:64, :], AF.Ln)
                        act(R_[64:128, :], O[0][64:128, :], AF.Ln)
                        act(R_, R_, AF.Exp, scale=-1.0)
                        psw = banks[7]
                        mm(psw, swp, R_)
                        tt("vector", RG_, psw, SGL, ALU.mult)
                        tt("vector", oT[0:64, c, G * GS:(G + 1) * GS], O[0][0:64, :], RG_[0:64, :], ALU.mult)
                        tt("vector", oT[64:128, c, G * GS:(G + 1) * GS], O[1][64:128, :], RG_[64:128, :], ALU.mult)
                    deferred.setdefault(step + 2, []).append(fin)
                    if G == NG - 1 and c + 2 < 8:
                        load_pair(c + 2)

            load_pair(0)
            load_pair(1)
            step = 0
            while step < n_items + D or any(k >= step for k in deferred):
                if step < n_items:
                    emitA(step)
                if 0 <= step - D < n_items:
                    emitBC(step - D, step)
                for f in deferred.pop(step, []):
                    f()
                step += 1
            S.barrier()
            off[0] = p4_mark
            set_banks(range(8))
            w_out = alloc([8, 1024], BF16, name="o_w_out")
            oov = od_w_out_b.rearrange("p (k n) -> p k n", k=8)
            for kc in range(0, 8, 4):
                load(w_out[:, kc:kc + 4, :], oov[:, kc:kc + 4, :])
            rbuf = [alloc([8, GS], F32, name=f"p5_r{i}") for i in range(3)]
            lt = (alloc([8, GS], BF16, name="p5_rb"), alloc([8, GS], BF16, name="p5_sq"),
                  alloc([GS], F32, name="p5_mean"), alloc([GS], F32, name="p5_rstd"))
            tA = [alloc([GS], F32, name=f"p5_tA{i}") for i in range(4)]
            srcv = x2T.rearrange("(kc p) t -> p kc t", p=128)
            dstv = x3T.rearrange("(kc p) t -> p kc t", p=128)
            if "d_oT" in dbg:
                for c in range(8):
                    for G in range(NTOK // GS):
                        cp("vector", rbuf[0][:, 0, :], oT[:, c, G * GS:(G + 1) * GS])
                        store(dbg_aps["d_oT"][c * 128:(c + 1) * 128, G * GS:(G + 1) * GS], rbuf[0][:, 0, :])
            NG = NTOK // GS
            load(rbuf[0], srcv[:, :, 0:GS])
            ui = 0
            prev = None
            for g in range(NG):
                t0 = g * GS
                R = rbuf[g % 3]
                for oc in range(8):
                    a_ = tA[ui % 4]
                    ui += 1
                    ps = pb()
                    for kc in range(8):
                        mm(ps, w_out[:, kc, oc * 128:(oc + 1) * 128], oT[:, kc, t0:t0 + GS], start=(kc == 0), stop=(kc == 7))
                    act(a_, ps, AF.Identity, scale=gate_(2, oc))
                    stt("vector", R[:, oc, :], R[:, oc, :], ALPHA, a_, ALU.mult, ALU.add)
                    if oc == 1 and prev is not None:
                        prev[1]()
                        prev[2]()
                    if oc == 3 and prev is not None:
                        prev[3]()
                    if oc == 5 and prev is not None:
                        prev[4]()
                        if g + 1 < NG:
                            load(rbuf[(g + 1) % 3], srcv[:, :, t0 + GS:t0 + 2 * GS])
                if g == 0 and NG > 1:
                    load(rbuf[1], srcv[:, :, GS:2 * GS])
                stg = ln_stages(R, R, 2, GS, lt, after=(lambda R=R, t0=t0: store(dstv[:, :, t0:t0 + GS], R)))
                stg[0]()
                prev = stg
            prev[1]()
            prev[2]()
            prev[3]()
            prev[4]()

        plan = [
            (1, phase1),
            (2, lambda: phase_mlp(0, x1T, x2T)),
            (3, phase3),
            (4, phase45),
            (5, lambda: phase_mlp(1, x3T, outT)),
        ]
        for pid, fn in plan:
            if pid > last_phase:
                break
            fn()
            S.barrier()
            off[0] = persist_mark
        S.finish()
        stats = {e: len(S.ops[e]) for e in S.ENG}
        stats["signal"] = S.n_signal
    return nc, stats


def _fm(v, nchunk):
    return np.ascontiguousarray(np.asarray(v, np.float32).reshape(nchunk, 128).T)


def _wl(w):
    K, N = w.shape
    return np.ascontiguousarray(np.asarray(w, np.float32).reshape(K // 128, 128, N).transpose(1, 0, 2))


def prepare_inputs(x, c, ada_w, ada_b, ln_g, ln_b, ev_w_in, ev_conv_w, ev_conv_b, ev_rg_wa, ev_rg_ba, ev_rg_wx,
                   ev_rg_bx, ev_rg_lam, ev_gla_w_up, ev_gla_b_up, ev_gla_norm_g, ev_w_out, od_w_in, od_b_f,
                   od_q_norm_g, od_k_norm_g, od_w_out, mlp_w1, mlp_b1, mlp_w2, mlp_b2):
    f = lambda a: np.ascontiguousarray(np.asarray(a, np.float32))
    shared = {
        "ada_w": np.stack([_wl(ada_w[l, j]) for l in range(2) for j in range(2)]),
        "ada_b": np.ascontiguousarray(np.stack([_fm(ada_b[l, j], 24) for l in range(2) for j in range(2)], axis=1)),
        "ln_g": np.ascontiguousarray(np.stack([_fm(ln_g[l, j], 8) for l in range(2) for j in range(2)], axis=1)),
        "ln_b": np.ascontiguousarray(np.stack([_fm(ln_b[l, j], 8) for l in range(2) for j in range(2)], axis=1)),
        "ev_w_in": _wl(ev_w_in[0]),
        "ev_conv_w": np.ascontiguousarray(np.asarray(ev_conv_w[0], np.float32).reshape(4, 4, 128).transpose(2, 1, 0)),
        "ev_conv_b": _fm(ev_conv_b[0], 4),
        "ev_rg_wa": f(ev_rg_wa[0]),
        "ev_rg_ba": _fm(ev_rg_ba[0], 4),
        "ev_rg_wx": f(ev_rg_wx[0]),
        "ev_rg_bx": _fm(ev_rg_bx[0], 4),
        "ev_rg_lam": _fm(ev_rg_lam[0], 4),
        "ev_gla_w_up": f(ev_gla_w_up[0]),
        "ev_gla_b_up": np.ascontiguousarray(np.asarray(ev_gla_b_up[0], np.float32).reshape(4, 64).T),
        "ev_gla_norm_g": _fm(ev_gla_norm_g[0], 4),
        "ev_w_out": _wl(ev_w_out[0]),
        "od_w_in": _wl(od_w_in[0]),
        "od_b_f": f(od_b_f[0]),
        "od_qk_g": np.ascontiguousarray(np.stack([np.tile(np.asarray(od_q_norm_g[0], np.float32), 2),
                                                  np.tile(np.asarray(od_k_norm_g[0], np.float32), 2)], axis=1)),
        "od_w_out": _wl(od_w_out[0]),
        "mlp_w1": np.stack([_wl(mlp_w1[l]) for l in range(2)]),
        "mlp_b1": np.ascontiguousarray(np.stack([_fm(mlp_b1[l], 32) for l in range(2)], axis=1)),
        "mlp_w2": np.stack([_wl(mlp_w2[l]) for l in range(2)]),
        "mlp_b2": np.ascontiguousarray(np.stack([_fm(mlp_b2[l], 8) for l in range(2)], axis=1)),
    }
    x = np.asarray(x, np.float32)
    c = np.asarray(c, np.float32)
    in_maps = []
    for b in range(x.shape[0]):
        m = dict(shared)
        m["xT"] = np.ascontiguousarray(x[b].T)
        m["c_fm"] = _fm(c[b], 8)
        in_maps.append(m)
    return in_maps


_CACHE = {}


def kernel(**inputs):
    in_maps = prepare_inputs(**inputs)
    if "nc" not in _CACHE:
        _CACHE["nc"] = build_program()[0]
    nc = _CACHE["nc"]
    res = run_bass_kernel_spmd(nc, in_maps, core_ids=list(range(len(in_maps))))
    out = np.stack([np.ascontiguousarray(np.asarray(r["outT"], np.float32).T) for r in res.results], axis=0)
    return out
```

```python
import numpy as np
from contextlib import ExitStack
import concourse.bass as bass
import concourse.mybir as mybir
from concourse.bass_utils import run_bass_kernel_spmd

F32 = mybir.dt.float32
BF16 = mybir.dt.bfloat16
U8 = mybir.dt.uint8
AF = mybir.ActivationFunctionType
ALU = mybir.AluOpType

SEM_CAP = 30000
NTOK = 4096
ALPHA = float((2 * 2) ** 0.25)


class Buf:
    __slots__ = ("name", "w", "r")

    def __init__(self, name):
        self.name = name
        self.w = None
        self.r = []


class V:
    __slots__ = ("buf", "ap")

    def __init__(self, buf, ap):
        self.buf = buf
        self.ap = ap

    def __getitem__(self, idx):
        return V(self.buf, self.ap[idx])

    def rearrange(self, *a, **k):
        return V(self.buf, self.ap.rearrange(*a, **k))

    def bc(self, shape):
        return V(self.buf, self.ap.to_broadcast(list(shape)))


class Sched:
    ENG = ("tensor", "vector", "scalar", "gpsimd", "sync")

    def __init__(self, nc, es):
        self.nc = nc
        self.es = es
        self.ops = {e: [] for e in self.ENG}
        self.same = {"vector", "scalar", "gpsimd"}
        self.dma_pool = {}
        self.dma_rr = {}
        self.n_dma_sems = 24
        self.sems = []
        self.pending = {e: [] for e in self.ENG}
        self.nbuf = 0

    def new_sem(self, name):
        s = self.es.enter_context(self.nc.semaphore(name))
        self.sems.append(s)
        return len(self.sems) - 1

    def view(self, ap, name=None):
        self.nbuf += 1
        return V(Buf(name or f"b{self.nbuf}"), ap)

    def _deps(self, eng, reads, writes):
        deps = []
        for b in reads:
            if b.w is not None:
                deps.append(b.w)
        for b in writes:
            if b.w is not None:
                deps.append(b.w)
            deps.extend(b.r)
        if eng not in self.same:
            deps = [d for d in deps if not (d[0] == "op" and d[1] == eng)]
        if self.pending[eng]:
            deps.extend(self.pending[eng])
            self.pending[eng] = []
        return deps

    def _mark(self, me, reads, writes):
        for b in reads:
            b.r.append(me)
        for b in writes:
            b.w = me
            b.r = []

    def op(self, eng, fn, reads=(), writes=()):
        deps = self._deps(eng, reads, writes)
        me = ("op", eng, len(self.ops[eng]))
        self.ops[eng].append({"fn": fn, "deps": deps, "dma": None})
        self._mark(me, reads, writes)
        return me

    def dma(self, eng, out_ap, in_ap, reads=(), writes=()):
        pool = self.dma_pool.setdefault(eng, [])
        if len(pool) < self.n_dma_sems:
            pool.append([self.new_sem(f"d_{eng}_{len(pool)}"), 0])
            slot = pool[-1]
        else:
            i = self.dma_rr.get(eng, 0)
            slot = pool[i % len(pool)]
            self.dma_rr[eng] = i + 1
        deps = self._deps(eng, reads, writes)
        if slot[1] > 0:
            deps.append(("dma", slot[0], slot[1]))
        slot[1] += 16
        me = ("dma", slot[0], slot[1])
        fn = lambda e, o=out_ap, i=in_ap: e.dma_start(out=o, in_=i)
        self.ops[eng].append({"fn": fn, "deps": deps, "dma": (slot[0], 16)})
        self._mark(me, reads, writes)
        return me

    def ins(self, eng, meth, _r=(), _w=(), **kw):
        reads, writes, real = list(_r), list(_w), {}
        for k, v in kw.items():
            if isinstance(v, V):
                (writes if k in ("out", "accum_out") else reads).append(v.buf)
                real[k] = v.ap
            else:
                real[k] = v
        import sys
        fr = sys._getframe(1)
        while fr.f_code.co_name in ("mm", "act", "tt", "ts", "stt", "cp", "memset") and fr.f_back is not None:
            fr = fr.f_back
        where = f"{fr.f_code.co_name}:{fr.f_lineno}"

        def fn(e, m=meth, r=real, where=where):
            try:
                return getattr(e, m)(**r)
            except BaseException as ex:
                raise RuntimeError(f"emit {m} at {where}: {str(ex)[:600]}") from None
        return self.op(eng, fn, reads, writes)

    def barrier(self):
        tg = []
        for e in self.ENG:
            for idx in range(len(self.ops[e]) - 1, -1, -1):
                if self.ops[e][idx]["dma"] is None and self.ops[e][idx]["fn"] is not None:
                    tg.append(("op", e, idx))
                    break
        for e, pool in self.dma_pool.items():
            for slot in pool:
                if slot[1] > 0:
                    tg.append(("dma", slot[0], slot[1]))
        for e in self.ENG:
            self.pending[e].extend(tg)

    def finish(self):
        self.barrier()
        self.ops["sync"].append({"fn": None, "deps": self._deps("sync", [], []), "dma": None})
        sig = {e: set() for e in self.ENG}
        for e in self.ENG:
            w_op = {}
            w_dma = {}
            for o in self.ops[e]:
                need_op, need_dma = {}, {}
                for d in o["deps"]:
                    if d[0] == "op":
                        if d[1] == e and e not in self.same:
                            continue
                        if need_op.get(d[1], -1) < d[2]:
                            need_op[d[1]] = d[2]
                    else:
                        if need_dma.get(d[1], 0) < d[2]:
                            need_dma[d[1]] = d[2]
                wl = []
                for pe, idx in need_op.items():
                    if w_op.get(pe, -1) < idx:
                        w_op[pe] = idx
                        wl.append(("op", pe, idx))
                        sig[pe].add(idx)
                for s, v in need_dma.items():
                    if w_dma.get(s, 0) < v:
                        w_dma[s] = v
                        wl.append(("dma", s, v))
                o["waits"] = wl
        semval = {}
        for e in self.ENG:
            n = 0
            cur = None
            for idx, o in enumerate(self.ops[e]):
                if idx in sig[e]:
                    if cur is None or n % SEM_CAP == 0:
                        cur = self.new_sem(f"s_{e}_{n // SEM_CAP}")
                    n += 1
                    semval[(e, idx)] = (cur, (n - 1) % SEM_CAP + 1)
        self.n_signal = {e: len(sig[e]) for e in self.ENG}
        nc = self.nc
        sems = self.sems
        with nc.Block() as block:
            def mk(engname):
                def body(e):
                    for idx, o in enumerate(self.ops[engname]):
                        for w in o["waits"]:
                            if w[0] == "op":
                                s, v = semval[(w[1], w[2])]
                            else:
                                s, v = w[1], w[2]
                            e.wait_ge(sems[s], v)
                        if o["fn"] is None:
                            continue
                        i = o["fn"](e)
                        if o["dma"] is not None:
                            i.then_inc(sems[o["dma"][0]], 16)
                        elif (engname, idx) in semval:
                            i.then_inc(sems[semval[(engname, idx)][0]], 1)
                return body
            for engname in self.ENG:
                if self.ops[engname]:
                    getattr(block, engname)(mk(engname))


def build_program(dbg=(), last_phase=5):
    nc = bass.Bass("TRN2", target_bir_lowering=False)

    def inp(name, shape):
        return nc.dram_tensor(name, list(shape), F32, kind="ExternalInput").ap()

    def scratch(name, shape, dt):
        kind = "ExternalOutput" if name in dbg else "Internal"
        return nc.dram_tensor(name, list(shape), dt, kind=kind).ap()

    xT = inp("xT", [1024, NTOK])
    c_fm = inp("c_fm", [128, 8])
    ada_w = inp("ada_w", [4, 128, 8, 3072])
    ada_b = inp("ada_b", [128, 4, 24])
    ln_g = inp("ln_g", [128, 4, 8])
    ln_b = inp("ln_b", [128, 4, 8])
    ev_w_in = inp("ev_w_in", [128, 8, 2576])
    ev_conv_w = inp("ev_conv_w", [128, 4, 4])
    ev_conv_b = inp("ev_conv_b", [128, 4])
    ev_rg_wa = inp("ev_rg_wa", [8, 64, 64])
    ev_rg_ba = inp("ev_rg_ba", [128, 4])
    ev_rg_wx = inp("ev_rg_wx", [8, 64, 64])
    ev_rg_bx = inp("ev_rg_bx", [128, 4])
    ev_rg_lam = inp("ev_rg_lam", [128, 4])
    ev_gla_w_up = inp("ev_gla_w_up", [16, 256])
    ev_gla_b_up = inp("ev_gla_b_up", [64, 4])
    ev_gla_norm_g = inp("ev_gla_norm_g", [128, 4])
    ev_w_out = inp("ev_w_out", [128, 8, 1024])
    od_w_in = inp("od_w_in", [128, 8, 4112])
    od_b_f = inp("od_b_f", [16])
    od_qk_g = inp("od_qk_g", [128, 2])
    od_w_out = inp("od_w_out", [128, 8, 1024])
    mlp_w1 = inp("mlp_w1", [2, 128, 8, 4096])
    mlp_b1 = inp("mlp_b1", [128, 2, 32])
    mlp_w2 = inp("mlp_w2", [2, 128, 32, 1024])
    mlp_b2 = inp("mlp_b2", [128, 2, 8])

    outT = nc.dram_tensor("outT", [1024, NTOK], F32, kind="ExternalOutput").ap()
    x1T = scratch("x1T", [1024, NTOK], F32)
    x2T = scratch("x2T", [1024, NTOK], F32)
    x3T = scratch("x3T", [1024, NTOK], F32)
    qT = scratch("qT", [1024, NTOK], BF16)
    kT = scratch("kT", [1024, NTOK], BF16)
    sgT = scratch("sgT", [1024, NTOK], F32)
    vaug = scratch("vaug", [16, NTOK, 128], BF16)
    w1b = [scratch(f"w1b{l}", [128, 8 * 4096], BF16) for l in range(2)]
    w2b = [scratch(f"w2b{l}", [128, 32 * 1024], BF16) for l in range(2)]
    od_w_in_b = scratch("od_w_in_b", [128, 8 * 4112], BF16)
    od_w_out_b = scratch("od_w_out_b", [128, 8 * 1024], BF16)
    dbg_aps = {}
    for nm, shape in (("d_mod", [128, 96]), ("d_mix", [1024, NTOK]), ("d_oT", [1024, NTOK]), ("d_L", [128, 512])):
        if nm in dbg:
            dbg_aps[nm] = nc.dram_tensor(nm, shape, F32, kind="ExternalOutput").ap()

    with ExitStack() as es:
        S = Sched(nc, es)
        ARENA = 207 * 1024
        arena = es.enter_context(nc.sbuf_tensor("arena", [128, ARENA], U8))
        off = [0]

        def alloc(shape, dt, nparts=128, name=None):
            n = int(np.prod(shape)) * mybir.dt.size(dt)
            n = (n + 31) // 32 * 32
            assert off[0] + n <= ARENA, f"SBUF arena overflow at {name}: {off[0] + n}"
            ap = arena[0:nparts, off[0]:off[0] + n].bitcast(dt)
            if int(np.prod(shape)) * mybir.dt.size(dt) != n:
                ap = ap[:, 0:int(np.prod(shape))]
            off[0] += n
            if len(shape) == 2:
                ap = ap.rearrange("p (a b) -> p a b", b=shape[1])
            elif len(shape) == 3:
                ap = ap.rearrange("p (a b c) -> p a b c", b=shape[1], c=shape[2])
            return S.view(ap, name)

        banks = [S.view(es.enter_context(nc.psum_tensor(f"pb{i}", [128, 512], F32))[:], f"bank{i}") for i in range(8)]
        rr = {"list": list(range(8)), "i": 0}

        def pb():
            b = banks[rr["list"][rr["i"] % len(rr["list"])]]
            rr["i"] += 1
            return b

        def set_banks(lst):
            rr["list"] = list(lst)
            rr["i"] = 0

        def mm(out, lhsT, rhs, start=True, stop=True):
            S.ins("tensor", "matmul", out=out, lhsT=lhsT, rhs=rhs, start=start, stop=stop)

        def act(out, in_, func, bias=None, scale=None):
            kw = {}
            if bias is not None:
                kw["bias"] = bias
            if scale is not None:
                kw["scale"] = scale
            S.ins("scalar", "activation", out=out, in_=in_, func=func, **kw)

        def tt(eng, out, in0, in1, op):
            S.ins(eng, "tensor_tensor", out=out, in0=in0, in1=in1, op=op)

        def ts(eng, out, in0, s1, s2, op0, op1=None):
            if op1 is None:
                S.ins(eng, "tensor_scalar", out=out, in0=in0, scalar1=s1, scalar2=None, op0=op0)
            else:
                S.ins(eng, "tensor_scalar", out=out, in0=in0, scalar1=s1, scalar2=s2, op0=op0, op1=op1)

        def stt(eng, out, in0, scalar, in1, op0, op1):
            S.ins(eng, "scalar_tensor_tensor", out=out, in0=in0, scalar=scalar, in1=in1, op0=op0, op1=op1)

        def cp(eng, out, in_):
            if eng == "scalar":
                act(out, in_, AF.Identity)
            else:
                S.ins(eng, "tensor_copy", out=out, in_=in_)

        def memset(eng, v, val):
            S.ins(eng, "memset", _w=[v.buf], ap=v.ap, constant=val)

        def load(v, src, eng="sync"):
            S.dma(eng, v.ap, src, writes=[v.buf])

        def store(dst, v, eng="sync"):
            S.dma(eng, dst, v.ap, reads=[v.buf])

        class ColBlocks:
            def __init__(self, full, bounds):
                self.bounds = list(bounds)
                self.blk = [V(Buf(f"{full.buf.name}_c{i}"), full.ap[:, :, bounds[i]:bounds[i + 1]])
                            for i in range(len(bounds) - 1)]

            def get(self, kc, c0, c1):
                for i in range(len(self.blk)):
                    if self.bounds[i] <= c0 and c1 <= self.bounds[i + 1]:
                        return self.blk[i][:, kc, c0 - self.bounds[i]:c1 - self.bounds[i]]
                raise AssertionError(f"column range {c0}:{c1} straddles blocks {self.bounds}")

        modv = alloc([4, 24], F32, name="modv")
        gb2 = alloc([2, 8], F32, name="gb2")
        lng = alloc([4, 8], F32, name="lng")
        lnb = alloc([4, 8], F32, name="lnb")
        b1_sb = alloc([2, 32], F32, name="b1")
        b2_sb = alloc([2, 8], F32, name="b2")
        ident = alloc([128], BF16, name="ident")
        onesD = alloc([128], BF16, name="onesD")
        eps_ln = alloc([1], F32, name="eps_ln")
        eps_rms = alloc([1], F32, name="eps_rms")
        L_tm = alloc([32, 16], F32, name="L_tm")
        cumtot = alloc([32, 16], F32, name="cumtot")

        load(lng, ln_g)
        load(lnb, ln_b)
        load(b1_sb, mlp_b1)
        load(b2_sb, mlp_b2)
        memset("gpsimd", ident, 0.0)
        S.ins("gpsimd", "affine_select", out=ident, in_=ident, pattern=[[-1, 128]], compare_op=ALU.not_equal,
              fill=1.0, base=0, channel_multiplier=1)
        memset("gpsimd", onesD, 1.0 / 1024.0)
        memset("gpsimd", eps_ln, 1e-5)
        memset("gpsimd", eps_rms, 1e-6)
        persist_mark = off[0]

        def ln_stages(r, out, li, T, tmp, after=None):
            rb, sq, mean_sb, rstd = tmp
            st = {}

            def T1():
                for kc in range(8):
                    cp("gpsimd" if kc % 2 == 0 else "vector", rb[:, kc, :], r[:, kc, :])
                    act(sq[:, kc, :], r[:, kc, :], AF.Square)

            def T2():
                st["m"] = pb()
                st["q"] = pb()
                for kc in range(8):
                    mm(st["m"][:, 0:T], onesD, rb[:, kc, :], start=(kc == 0), stop=(kc == 7))
                for kc in range(8):
                    mm(st["q"][:, 0:T], onesD, sq[:, kc, :], start=(kc == 0), stop=(kc == 7))

            def T3():
                cp("vector", mean_sb, st["m"][:, 0:T])
                tt("vector", rstd, mean_sb, mean_sb, ALU.mult)
                tt("vector", rstd, st["q"][:, 0:T], rstd, ALU.subtract)
                act(rstd, rstd, AF.Ln, bias=eps_ln[:, 0:1])
                act(rstd, rstd, AF.Exp, scale=-0.5)

            def T4a():
                for kc in range(8):
                    tt("vector", out[:, kc, :], r[:, kc, :], mean_sb, ALU.subtract)
                    tt("gpsimd" if kc % 2 == 0 else "vector", out[:, kc, :], out[:, kc, :], rstd, ALU.mult)

            def T4b():
                for kc in range(8):
                    act(out[:, kc, :], out[:, kc, :], AF.Identity, bias=lnb[:, li, kc:kc + 1], scale=lng[:, li, kc:kc + 1])
                if after is not None:
                    after()
            return T1, T2, T3, T4a, T4b


        set_banks(range(8))
        c_sb = alloc([8], F32, name="c_sb")
        sc_bf = alloc([8], BF16, name="sc_bf")
        adab_sb = alloc([4, 24], F32, name="adab")
        wada = [alloc([8, 768], BF16, name=f"wada{i}") for i in range(2)]
        load(c_sb, c_fm)
        load(adab_sb, ada_b)
        act(sc_bf, c_sb, AF.Silu)
        ps_ada = pb()
        for a in range(4):
            for q in range(4):
                slot = wada[(a * 4 + q) % 2]
                load(slot, ada_w[a][:, :, q * 768:(q + 1) * 768], eng="gpsimd")
                for oc in range(6):
                    col = a * 24 + q * 6 + oc
                    for kc in range(8):
                        mm(ps_ada[:, col:col + 1], slot[:, kc, oc * 128:(oc + 1) * 128], sc_bf[:, kc:kc + 1],
                           start=(kc == 0), stop=(kc == 7))
        tt("vector", modv, ps_ada[:, 0:96].rearrange("p (a b) -> p a b", b=24), adab_sb, ALU.add)
        ts("vector", modv[:, :, 8:24], modv[:, :, 8:24], 1.0, None, ALU.add)
        for l in range(2):
            tt("vector", gb2[:, l, :], modv[:, 2 * l + 1, 16:24], b2_sb[:, l, :], ALU.mult)
        if "d_mod" in dbg:
            store(dbg_aps["d_mod"], modv.rearrange("p a b -> p (a b)"))
        S.barrier()
        off[0] = persist_mark

        def shift_(a, kc):
            return modv[:, a, kc:kc + 1]

        def scale_(a, kc):
            return modv[:, a, 8 + kc:9 + kc]

        def gate_(a, kc):
            return modv[:, a, 16 + kc:17 + kc]

        def phase1():
            set_banks(range(8))
            GS = 256
            w_in = alloc([8, 2576], BF16, name="w_in")
            w_out = alloc([8, 1024], BF16, name="w_out")
            w_inB = ColBlocks(w_in, [0, 512, 1024, 1536, 2048, 2576])
            for i in range(5):
                b0, b1 = w_inB.bounds[i], w_inB.bounds[i + 1]
                load(w_inB.blk[i], ev_w_in[:, :, b0:b1], eng="gpsimd")
            for kc in range(0, 8, 4):
                load(w_out[:, kc:kc + 4, :], ev_w_out[:, kc:kc + 4, :], eng="gpsimd")
            convw = alloc([4, 4], F32, name="convw")
            convb = alloc([4], F32, name="convb")
            ba = alloc([4], F32, name="ba")
            bx = alloc([4], F32, name="bx")
            lam = alloc([4], F32, name="lam")
            cl = alloc([4], F32, name="cl")
            cl2 = alloc([4], F32, name="cl2")
            nbup = alloc([4], F32, nparts=64, name="nbup")
            gng = alloc([4], F32, name="gng")
            load(convw, ev_conv_w)
            load(convb, ev_conv_b)
            load(ba, ev_rg_ba)
            load(bx, ev_rg_bx)
            load(lam, ev_rg_lam)
            load(nbup, ev_gla_b_up)
            load(gng, ev_gla_norm_g)
            ts("vector", nbup, nbup, -1.0, None, ALU.mult)
            act(cl, lam, AF.Exp, scale=-1.0)
            act(cl, cl, AF.Ln, bias=1.0)
            ts("vector", cl2, cl, -16.0, None, ALU.mult)
            ts("vector", cl, cl, -8.0, None, ALU.mult)
            wa_f = alloc([4, 128], F32, name="wa_f")
            wx_f = alloc([4, 128], F32, name="wx_f")
            wa_bd = alloc([4, 128], BF16, name="wa_bd")
            wx_bd = alloc([4, 128], BF16, name="wx_bd")
            memset("gpsimd", wa_f, 0.0)
            memset("gpsimd", wx_f, 0.0)
            for cc in range(4):
                for e in range(2):
                    S.dma("sync", wa_f.ap[e * 64:(e + 1) * 64, cc, e * 64:(e + 1) * 64], ev_rg_wa[2 * cc + e], writes=[wa_f.buf])
                    S.dma("sync", wx_f.ap[e * 64:(e + 1) * 64, cc, e * 64:(e + 1) * 64], ev_rg_wx[2 * cc + e], writes=[wx_f.buf])
            cp("vector", wa_bd, wa_f)
            cp("vector", wx_bd, wx_f)
            wup_f = alloc([256], F32, nparts=16, name="wup_f")
            wup_bf = alloc([256], BF16, nparts=16, name="wup_bf")
            load(wup_f, ev_gla_w_up)
            cp("vector", wup_bf, wup_f)
            ones128 = alloc([128], BF16, name="ones128")
            memset("gpsimd", ones128, 1.0 / 128.0)
            mask4 = alloc([4, 64], F32, nparts=64, name="mask4")
            memset("gpsimd", mask4, 1.0)
            S.ins("gpsimd", "affine_select", out=mask4, in_=mask4, pattern=[[0, 4], [1, 64]], compare_op=ALU.is_ge,
                  fill=0.0, base=0, channel_multiplier=-1)
            cmask = alloc([4 * GS // 64, 64], F32, nparts=64, name="cmask")
            memset("gpsimd", cmask, 1.0)
            memset("gpsimd", cmask[:, :, 0:1], 0.0)
            Sst = alloc([4, 128], F32, nparts=64, name="Sst")
            Sbf = alloc([4, 128], BF16, nparts=64, name="Sbf")
            memset("gpsimd", Sst, 0.0)
            memset("gpsimd", Sbf, 0.0)
            hlast = alloc([4], F32, name="hlast")
            memset("gpsimd", hlast, 0.0)
            xg = [alloc([8, GS], F32, name=f"xg{i}") for i in range(2)]
            ug = [alloc([8, GS], BF16, name=f"ug{i}") for i in range(2)]
            xr = [[alloc([GS + 3], F32, name=f"xr{i}_{cc}") for cc in range(4)] for i in range(2)]
            for cc in range(4):
                memset("gpsimd", xr[1][cc][:, GS:GS + 3], 0.0)
            mix = alloc([8, GS], BF16, name="mix")
            rbuf = [alloc([8, GS], F32, name=f"rbuf{i}") for i in range(2)]
            lt = (alloc([8, GS], BF16, name="ln_rb"), alloc([8, GS], BF16, name="ln_sq"),
                  alloc([GS], F32, name="ln_mean"), alloc([GS], F32, name="ln_rstd"))
            tR = [{k: alloc([GS], F32, name=f"t{k}{cc}") for k in "abcde"} for cc in range(4)]
            xcb = [alloc([GS], BF16, name=f"xcb{cc}") for cc in range(4)]
            tO = [alloc([GS], F32, name=f"tO{i}") for i in range(3)]
            q_sb = alloc([4, GS], F32, nparts=64, name="q_sb")
            k_sb = alloc([4, GS], F32, nparts=64, name="k_sb")
            l_sb = alloc([4, GS], F32, nparts=64, name="l_sb")
            cs_sb = alloc([4, GS], F32, nparts=64, name="cs_sb")
            eb = alloc([4, GS], F32, nparts=64, name="eb")
            enb = alloc([4, GS], F32, nparts=64, name="enb")
            qd = alloc([4, GS], BF16, nparts=64, name="qd")
            kd = alloc([4, GS], BF16, nparts=64, name="kd")
            zl_bf = alloc([GS], BF16, nparts=16, name="zl_bf")
            v_n = [alloc([512], BF16, nparts=64, name=f"v_n{i}") for i in range(2)]
            kd_n = [alloc([256], BF16, nparts=64, name=f"kd_n{i}") for i in range(2)]
            att_sb = [alloc([4, 64], BF16, nparts=64, name=f"att{i}") for i in range(2)]
            o_g = alloc([4, GS], F32, name="o_g")
            osq = [alloc([GS], BF16, name=f"osq{h}") for h in range(4)]
            tmpS = alloc([4, 128], F32, nparts=64, name="tmpS")

            NG = NTOK // GS
            NCH = GS // 64
            xv = xT.rearrange("(kc p) t -> p kc t", p=128)
            def bg_cast(dst, src2d, npieces):
                n = src2d.shape[1]
                step = n // npieces
                for i in range(npieces):
                    S.dma("gpsimd", dst[:, i * step:(i + 1) * step], src2d[:, i * step:(i + 1) * step])

            def p1_make_u(g):
                for kc in range(8):
                    act(ug[g % 2][:, kc, :], xg[g % 2][:, kc, :], AF.Identity, bias=shift_(0, kc), scale=scale_(0, kc))

            load(xg[0], xv[:, :, 0:GS])
            prev = None
            oi = 0
            for g in range(NG):
                t0 = g * GS
                X = xg[g % 2]
                U = ug[g % 2]
                if g + 1 < NG:
                    load(xg[(g + 1) % 2], xv[:, :, t0 + GS:t0 + 2 * GS])
                if g == 0:
                    p1_make_u(0)
                if g == 1:
                    bg_cast(w1b[0], mlp_w1[0].rearrange("p k n -> p (k n)"), 4)
                    bg_cast(w2b[0], mlp_w2[0].rearrange("p k n -> p (k n)"), 4)
                if g == 5:
                    bg_cast(od_w_in_b, od_w_in.rearrange("p k n -> p (k n)"), 4)
                    bg_cast(od_w_out_b, od_w_out.rearrange("p k n -> p (k n)"), 1)
                if g == 8:
                    bg_cast(w1b[1], mlp_w1[1].rearrange("p k n -> p (k n)"), 4)
                    bg_cast(w2b[1], mlp_w2[1].rearrange("p k n -> p (k n)"), 4)
                XR = [xr[g % 2][cc] for cc in range(4)]
                XRp = [xr[(g + 1) % 2][cc] for cc in range(4)]
                for cc in range(4):
                    ps = pb()[:, 0:GS]
                    for kc in range(8):
                        mm(ps, w_inB.get(kc, cc * 128, (cc + 1) * 128), U[:, kc, :], start=(kc == 0), stop=(kc == 7))
                    cp("scalar", XR[cc][:, 3:GS + 3], ps)
                    cp("gpsimd", XR[cc][:, 0:3], XRp[cc][:, GS:GS + 3])
                for cc in range(4):
                    ps2 = pb()[:, 0:GS]
                    for kc in range(8):
                        mm(ps2, w_inB.get(kc, 512 + cc * 128, 512 + (cc + 1) * 128), U[:, kc, :], start=(kc == 0), stop=(kc == 7))
                    act(tR[cc]["e"], ps2, AF.Gelu_apprx_tanh)
                if prev is not None:
                    prev[1]()
                    prev[2]()
                    prev[3]()
                for cc in range(4):
                    ts("vector", tR[cc]["a"], XR[cc][:, 3:GS + 3], convw[:, cc, 3:4], convb[:, cc:cc + 1], ALU.mult, ALU.add)
                for tap in (2, 1, 0):
                    for cc in range(4):
                        stt("vector", tR[cc]["a"], XR[cc][:, tap:GS + tap], convw[:, cc, tap:tap + 1], tR[cc]["a"], ALU.mult, ALU.add)
                for cc in range(4):
                    cp("gpsimd", xcb[cc], tR[cc]["a"])
                psr, psi = [], []
                for cc in range(4):
                    p1 = pb()[:, 0:GS]
                    mm(p1, wa_bd[:, cc, :], xcb[cc])
                    p2 = pb()[:, 0:GS]
                    mm(p2, wx_bd[:, cc, :], xcb[cc])
                    psr.append(p1)
                    psi.append(p2)
                for cc in range(4):
                    act(tR[cc]["b"], psr[cc], AF.Sigmoid, bias=ba[:, cc:cc + 1])
                    act(tR[cc]["c"], psi[cc], AF.Sigmoid, bias=bx[:, cc:cc + 1])
                for cc in range(4):
                    act(tR[cc]["d"], tR[cc]["b"], AF.Exp, scale=cl2[:, cc:cc + 1])
                    act(tR[cc]["b"], tR[cc]["b"], AF.Exp, scale=cl[:, cc:cc + 1])
                for cc in range(4):
                    act(tR[cc]["d"], tR[cc]["d"], AF.Sqrt, bias=1.0, scale=-1.0)
                    tt("gpsimd", tR[cc]["c"], tR[cc]["c"], tR[cc]["a"], ALU.mult)
                for h in range(4):
                    ps = pb()[:, 0:GS]
                    for kc in range(8):
                        mm(ps[0:64, :], w_inB.get(kc, 1024 + h * 64, 1024 + (h + 1) * 64), U[:, kc, :], start=(kc == 0), stop=(kc == 7))
                    cp("scalar", q_sb[:, h, :], ps[0:64, :])
                    ps = pb()[:, 0:GS]
                    for kc in range(8):
                        mm(ps[0:64, :], w_inB.get(kc, 1280 + h * 64, 1280 + (h + 1) * 64), U[:, kc, :], start=(kc == 0), stop=(kc == 7))
                    cp("scalar", k_sb[:, h, :], ps[0:64, :])
                ps = pb()[:, 0:GS]
                for kc in range(8):
                    mm(ps[0:16, :], w_inB.get(kc, 2560, 2576), U[:, kc, :], start=(kc == 0), stop=(kc == 7))
                cp("scalar", zl_bf, ps[0:16, :])
                for cc in range(4):
                    tt("vector", tR[cc]["c"], tR[cc]["c"], tR[cc]["d"], ALU.mult)
                for cc in range(4):
                    S.ins("vector", "tensor_tensor_scan", out=tR[cc]["d"], data0=tR[cc]["b"], data1=tR[cc]["c"],
                          initial=hlast[:, cc:cc + 1], op0=ALU.mult, op1=ALU.add)
                for cc in range(4):
                    cp("vector", hlast[:, cc:cc + 1], tR[cc]["d"][:, GS - 1:GS])
                    tt("gpsimd", mix[:, cc, :], tR[cc]["d"], tR[cc]["e"], ALU.mult)
                if prev is not None:
                    prev[4]()
                for h in range(4):
                    ps = pb()[:, 0:GS]
                    mm(ps[0:64, :], wup_bf[:, h * 64:(h + 1) * 64], zl_bf)
                    act(l_sb[:, h, :], ps[0:64, :], AF.Exp, bias=nbup[:, h:h + 1], scale=-1.0)
                act(l_sb, l_sb, AF.Ln, bias=1.0)
                S.ins("vector", "tensor_tensor_scan", out=cs_sb.rearrange("p h t -> p (h t)"),
                      data0=cmask.rearrange("p a b -> p (a b)"), data1=l_sb.rearrange("p h t -> p (h t)"),
                      initial=0.0, op0=ALU.mult, op1=ALU.add)
                act(eb, cs_sb, AF.Exp, scale=-1.0 / 16.0)
                act(enb, cs_sb, AF.Exp, scale=1.0 / 16.0)
                stt("vector", qd, q_sb, 0.125, eb, ALU.mult, ALU.mult)
                tt("gpsimd", kd, k_sb, enb, ALU.mult)
                sgate = []
                for h in range(4):
                    psg = pb()[:, 0:GS]
                    for kc in range(8):
                        mm(psg, w_inB.get(kc, 2048 + h * 128, 2048 + (h + 1) * 128), U[:, kc, :], start=(kc == 0), stop=(kc == 7))
                    act(tR[h]["b"], psg, AF.Silu)
                    sgate.append(tR[h]["b"])

                def chunk_front(n):
                    tc = n * 64
                    VN, KN, AT = v_n[n % 2], kd_n[n % 2], att_sb[n % 2]
                    psv = pb()
                    for kc in range(8):
                        mm(psv[0:64, :], U[:, kc, tc:tc + 64], w_inB.get(kc, 1536, 2048), start=(kc == 0), stop=(kc == 7))
                    cp("scalar", VN, psv[0:64, :])
                    pst = pb()
                    pst_bf = V(pst.buf, pst.ap.bitcast(BF16))
                    for h in range(4):
                        S.ins("tensor", "transpose", out=pst_bf[0:64, h * 64:(h + 1) * 64], in_=kd[:, h, tc:tc + 64],
                              identity=ident[0:64, 0:64])
                    cp("vector", KN, pst_bf[0:64, 0:256])
                    psa = pb()
                    for h in range(4):
                        mm(psa[0:64, h * 64:(h + 1) * 64], kd[:, h, tc:tc + 64], qd[:, h, tc:tc + 64])
                    tt("vector", AT, psa[0:64, 0:256].rearrange("p (h t) -> p h t", h=4), mask4, ALU.mult)

                def chunk_back(n):
                    tc = n * 64
                    VN, KN, AT = v_n[n % 2], kd_n[n % 2], att_sb[n % 2]
                    pso = pb()
                    for h in range(4):
                        mm(pso[:, h * 64:(h + 1) * 64], VN[:, h * 128:(h + 1) * 128], AT[:, h, :], start=True, stop=False)
                        mm(pso[:, h * 64:(h + 1) * 64], Sbf[:, h, :], qd[:, h, tc:tc + 64], start=False, stop=True)
                    cp("scalar", o_g[:, :, tc:tc + 64], pso[:, 0:256].rearrange("p (h t) -> p h t", h=4))
                    psk = pb()
                    for h in range(4):
                        mm(psk[0:64, h * 128:(h + 1) * 128], KN[:, h * 64:(h + 1) * 64], VN[:, h * 128:(h + 1) * 128])
                    tt("vector", tmpS, Sst, psk[0:64, :].rearrange("p (h d) -> p h d", h=4), ALU.add)
                    dec = eb[:, :, tc + 63:tc + 64].bc([64, 4, 128])
                    tt("vector", Sst, tmpS, dec, ALU.mult)
                    cp("gpsimd", Sbf, Sst)

                if g + 1 < NG:
                    p1_make_u(g + 1)
                chunk_front(0)
                for n in range(NCH):
                    if n + 1 < NCH:
                        chunk_front(n + 1)
                    chunk_back(n)
                psm = []
                for h in range(4):
                    tt("gpsimd", osq[h], o_g[:, h, :], o_g[:, h, :], ALU.mult)
                for h in range(4):
                    p = pb()[:, 0:GS]
                    mm(p, ones128, osq[h])
                    psm.append(p)
                for h in range(4):
                    act(tR[h]["a"], psm[h], AF.Ln, bias=eps_rms[:, 0:1])
                for h in range(4):
                    act(tR[h]["a"], tR[h]["a"], AF.Exp, scale=-0.5)
                for h in range(4):
                    stt("vector", tR[h]["c"], o_g[:, h, :], gng[:, h:h + 1], tR[h]["a"], ALU.mult, ALU.mult)
                for h in range(4):
                    tt("gpsimd", mix[:, 4 + h, :], tR[h]["c"], sgate[h], ALU.mult)
                if "d_mix" in dbg:
                    tmpf = rbuf[(g + 1) % 2]
                    cp("vector", tmpf, mix)
                    store(dbg_aps["d_mix"].rearrange("(kc p) t -> p kc t", p=128)[:, :, t0:t0 + GS], tmpf)
                R = rbuf[g % 2]
                for oc in range(8):
                    a_ = tO[oi % 3]
                    oi += 1
                    ps = pb()[:, 0:GS]
                    for kc in range(8):
                        mm(ps, w_out[:, kc, oc * 128:(oc + 1) * 128], mix[:, kc, :], start=(kc == 0), stop=(kc == 7))
                    act(a_, ps, AF.Identity, scale=gate_(0, oc))
                    stt("vector", R[:, oc, :], X[:, oc, :], ALPHA, a_, ALU.mult, ALU.add)
                stg = ln_stages(R, R, 0, GS, lt, after=(lambda R=R, t0=t0: store(x1T.rearrange("(kc p) t -> p kc t", p=128)[:, :, t0:t0 + GS], R)))
                stg[0]()
                prev = stg
            prev[1]()
            prev[2]()
            prev[3]()
            prev[4]()

        def phase_mlp(l, src, dst):
            set_banks(range(8))
            GS = 256
            a_idx = 2 * l + 1
            li = 2 * l + 1
            w1 = alloc([8, 4096], BF16, name="w1")
            w2 = alloc([32, 1024], BF16, name="w2")
            ug = [alloc([8, GS], BF16, name=f"m_ug{i}") for i in range(2)]
            hT_all = alloc([32, GS], BF16, name="m_hT")
            hT = [V(Buf(f"m_hT{fc}"), hT_all.ap[:, fc, :]) for fc in range(32)]
            rbuf = [alloc([8, GS], F32, name=f"m_r{i}") for i in range(3)]
            lt = (alloc([8, GS], BF16, name="m_rb"), alloc([8, GS], BF16, name="m_sq"),
                  alloc([GS], F32, name="m_mean"), alloc([GS], F32, name="m_rstd"))
            NT = 12
            tA = [alloc([GS], F32, name=f"m_tA{i}") for i in range(NT)]
            srcv = src.rearrange("(kc p) t -> p kc t", p=128)
            dstv = dst.rearrange("(kc p) t -> p kc t", p=128)
            NG = NTOK // GS
            load(rbuf[0], srcv[:, :, 0:GS])
            w1v = w1b[l].rearrange("p (k n) -> p k n", k=8)
            w2v = w2b[l].rearrange("p (k n) -> p k n", k=32)
            w1B = ColBlocks(w1, [j * 512 for j in range(9)])
            w2P = [V(Buf(f"w2_r{j}"), w2.ap[:, j * 4:(j + 1) * 4, :]) for j in range(8)]
            for j in range(8):
                load(w1B.blk[j], w1v[:, :, j * 512:(j + 1) * 512])
            for j in range(8):
                load(w2P[j], w2v[:, j * 4:(j + 1) * 4, :])
            ui = [0]
            prev = None

            def fc_block(U, lo, hi):
                for fc in range(lo, hi):
                    a_ = tA[ui[0] % NT]
                    ui[0] += 1
                    ps = pb()
                    for kc in range(8):
                        mm(ps[:, 0:GS], w1B.get(kc, fc * 128, (fc + 1) * 128), U[:, kc, :], start=(kc == 0), stop=(kc == 7))
                    act(a_, ps[:, 0:GS], AF.Relu, bias=b1_sb[:, l, fc:fc + 1])
                    tt("vector" if fc % 2 == 0 else "gpsimd", hT[fc], a_, a_, ALU.mult)

            def make_u(g):
                for kc in range(8):
                    act(ug[g % 2][:, kc, :], rbuf[g % 3][:, kc, :], AF.Identity, bias=shift_(a_idx, kc), scale=scale_(a_idx, kc))

            make_u(0)
            if NG > 1:
                load(rbuf[1], srcv[:, :, GS:2 * GS])
            for g in range(NG):
                t0 = g * GS
                R = rbuf[g % 3]
                U = ug[g % 2]
                fc_block(U, 0, 8)
                if prev is not None:
                    prev[1]()
                    prev[2]()
                fc_block(U, 8, 14)
                if prev is not None:
                    prev[3]()
                fc_block(U, 14, 20)
                if prev is not None:
                    prev[4]()
                if g + 2 < NG:
                    load(rbuf[(g + 2) % 3], srcv[:, :, t0 + 2 * GS:t0 + 3 * GS])
                fc_block(U, 20, 26)
                if g + 1 < NG:
                    make_u(g + 1)
                fc_block(U, 26, 32)
                for oc in range(8):
                    a_ = tA[ui[0] % NT]
                    ui[0] += 1
                    ps = pb()
                    for fc in range(32):
                        mm(ps[:, 0:GS], w2P[fc // 4][:, fc % 4, oc * 128:(oc + 1) * 128], hT[fc], start=(fc == 0), stop=(fc == 31))
                    act(a_, ps[:, 0:GS], AF.Identity, bias=gb2[:, l, oc:oc + 1], scale=gate_(a_idx, oc))
                    stt("vector", R[:, oc, :], R[:, oc, :], ALPHA, a_, ALU.mult, ALU.add)
                stg = ln_stages(R, R, li, GS, lt, after=(lambda R=R, t0=t0: store(dstv[:, :, t0:t0 + GS], R)))
                stg[0]()
                prev = stg
            prev[1]()
            prev[2]()
            prev[3]()
            prev[4]()

        def phase3():
            set_banks(range(7))
            flb = banks[7]
            GS = 512
            w_in = alloc([8, 4112], BF16, name="o_w_in")
            owv = od_w_in_b.rearrange("p (k n) -> p k n", k=8)
            w_inB = ColBlocks(w_in, [0, 512, 1024, 1536, 2048, 2560, 3072, 3584, 4112])
            for i in range(8):
                b0, b1 = w_inB.bounds[i], w_inB.bounds[i + 1]
                load(w_inB.blk[i], owv[:, :, b0:b1])
            bd64 = alloc([128], BF16, name="bd64")
            memset("gpsimd", bd64, 0.0)
            memset("gpsimd", bd64[0:64, 0:64], 1.0 / 64.0)
            memset("gpsimd", bd64[64:128, 64:128], 1.0 / 64.0)
            qkg = alloc([2], F32, name="qkg")
            load(qkg, od_qk_g)
            ts("vector", qkg[:, 0:1], qkg[:, 0:1], 0.125, None, ALU.mult)
            bf_bc = alloc([16], F32, name="bf_bc")
            load(bf_bc, od_b_f.partition_broadcast(128))
            triU = alloc([128], F32, name="triU")
            memset("gpsimd", triU, 1.0)
            S.ins("gpsimd", "affine_select", out=triU, in_=triU, pattern=[[1, 128]], compare_op=ALU.is_ge,
                  fill=0.0, base=0, channel_multiplier=-1)
            onesF = alloc([128], F32, name="onesF")
            memset("gpsimd", onesF, 1.0)
            ones32 = alloc([32], F32, name="ones32")
            memset("gpsimd", ones32, 1.0)
            xg = [alloc([8, GS], F32, name=f"p3_xg{i}") for i in range(2)]
            ug = [alloc([8, GS], BF16, name=f"p3_ug{i}") for i in range(2)]
            NT = 4
            tA = [alloc([GS], F32, name=f"p3_tA{i}") for i in range(NT)]
            tB = [alloc([GS], F32, name=f"p3_tB{i}") for i in range(NT)]
            sqb = [alloc([GS], BF16, name=f"p3_sq{i}") for i in range(NT)]
            qkout = [alloc([8, GS], BF16, name=f"p3_qk{i}") for i in range(2)]
            sgout = [alloc([GS], F32, name=f"p3_sg{i}") for i in range(3)]
            vst = [alloc([16, 128], BF16, name=f"p3_vst{i}") for i in range(2)]
            for i in range(2):
                memset("gpsimd", vst[i], 1.0)
            srcv = x2T.rearrange("(kc p) t -> p kc t", p=128)
            load(xg[0], srcv[:, :, 0:GS])
            ui = 0
            si = 0
            for g in range(NTOK // GS):
                t0 = g * GS
                X = xg[g % 2]
                U = ug[g % 2]
                if g + 1 < NTOK // GS:
                    load(xg[(g + 1) % 2], srcv[:, :, t0 + GS:t0 + 2 * GS])
                for kc in range(8):
                    act(U[:, kc, :], X[:, kc, :], AF.Identity, bias=shift_(2, kc), scale=scale_(2, kc))
                def qk_A(k):
                    which, c = divmod(k, 8)
                    ps = pb()
                    col = which * 1024 + c * 128
                    for kc in range(8):
                        mm(ps, w_inB.get(kc, col, col + 128), U[:, kc, :], start=(kc == 0), stop=(kc == 7))
                    a_, s_ = tA[k % NT], sqb[k % NT]
                    cp("scalar", a_, ps)
                    tt("gpsimd", s_, a_, a_, ALU.mult)

                def qk_C(k):
                    which, c = divmod(k, 8)
                    a_, b_, s_ = tA[k % NT], tB[k % NT], sqb[k % NT]
                    psm = pb()
                    mm(psm, bd64, s_)
                    act(b_, psm, AF.Ln, bias=eps_rms[:, 0:1])
                    act(b_, b_, AF.Exp, scale=-0.5)
                    stt("vector", qkout[which][:, c, :], a_, qkg[:, which:which + 1], b_, ALU.mult, ALU.mult)
                    if c == 7:
                        store((qT if which == 0 else kT).rearrange("(c p) t -> p c t", p=128)[:, :, t0:t0 + GS], qkout[which])

                for step in range(16 + 2):
                    if step < 16:
                        qk_A(step)
                    if step >= 2:
                        qk_C(step - 2)
                for c in range(8):
                    sg_ = sgout[si % 3]
                    si += 1
                    ps = pb()
                    col = 3072 + c * 128
                    for kc in range(8):
                        mm(ps, w_inB.get(kc, col, col + 128), U[:, kc, :], start=(kc == 0), stop=(kc == 7))
                    act(sg_, ps, AF.Sigmoid)
                    store(sgT[c * 128:(c + 1) * 128, t0:t0 + GS], sg_)
                for tt_ in range(4):
                    j = g * 4 + tt_
                    VS = vst[j % 2]
                    VS4 = VS.rearrange("p (c e) d -> p c e d", e=2)
                    for half in range(2):
                        ps = pb()
                        col = 2048 + half * 512
                        for kc in range(8):
                            mm(ps, U[:, kc, tt_ * 128:(tt_ + 1) * 128], w_inB.get(kc, col, col + 512), start=(kc == 0), stop=(kc == 7))
                        psv = ps.rearrange("p (c e d) -> p c e d", c=4, e=2)
                        cp("scalar", VS4[:, half * 4:(half + 1) * 4, 0, 0:64], psv[:, :, 0, :])
                        cp("vector", VS4[:, half * 4:(half + 1) * 4, 1, 64:128], psv[:, :, 1, :])
                    store(vaug.rearrange("h t d -> t h d")[j * 128:(j + 1) * 128, :, :], VS)
                    for kc in range(8):
                        mm(flb[:, j * 16:(j + 1) * 16], U[:, kc, tt_ * 128:(tt_ + 1) * 128], w_inB.get(kc, 4096, 4112),
                           start=(kc == 0), stop=(kc == 7))
            z = alloc([32, 16], F32, name="p3_z")
            tot = alloc([32, 16], F32, name="p3_tot")
            tt("vector", z, flb.rearrange("p (j h) -> p j h", h=16), bf_bc.rearrange("p (o h) -> p o h", o=1).bc([128, 32, 16]), ALU.add)
            act(z, z, AF.Exp, scale=-1.0)
            act(z, z, AF.Ln, bias=1.0)
            zf = z.rearrange("p j h -> p (j h)")
            ps_cs = pb()
            mm(ps_cs, triU, zf)
            ps_tot = pb()
            mm(ps_tot, onesF, zf)
            cp("scalar", tot, ps_tot.rearrange("p (j h) -> p j h", h=16))
            for h in range(16):
                S.ins("vector", "tensor_tensor_scan", out=cumtot[:, :, h], data0=ones32, data1=tot[:, :, h], initial=0.0,
                      op0=ALU.mult, op1=ALU.add)
            tt("vector", tot, cumtot, tot, ALU.subtract)
            tt("vector", L_tm, ps_cs.rearrange("p (j h) -> p j h", h=16), tot, ALU.add)
            if "d_L" in dbg:
                store(dbg_aps["d_L"], L_tm.rearrange("p j h -> p (j h)"))

        def phase45():
            GS = 512
            oT = alloc([8, NTOK], BF16, name="oT")
            swp = alloc([128], F32, name="swp")
            memset("gpsimd", swp, 0.0)
            S.ins("gpsimd", "affine_select", out=swp[:, 0:64], in_=swp[:, 0:64], pattern=[[-1, 64]], compare_op=ALU.not_equal,
                  fill=1.0, base=-64, channel_multiplier=1)
            S.ins("gpsimd", "affine_select", out=swp[:, 64:128], in_=swp[:, 64:128], pattern=[[-1, 64]], compare_op=ALU.not_equal,
                  fill=1.0, base=0, channel_multiplier=1)
            negmask = alloc([128], BF16, name="negmask")
            memset("gpsimd", negmask, -30000.0)
            S.ins("gpsimd", "affine_select", out=negmask, in_=negmask, pattern=[[-1, 128]], compare_op=ALU.is_gt,
                  fill=0.0, base=0, channel_multiplier=1)
            p4_mark = off[0]
            qc = [[alloc([NTOK], BF16, name=f"a_q{i}_{e}") for e in range(2)] for i in range(2)]
            for i in range(2):
                memset("gpsimd", qc[i][0][64:128, :], 0.0)
                memset("gpsimd", qc[i][1][0:64, :], 0.0)
            kc_ = [alloc([NTOK], BF16, name=f"a_k{i}") for i in range(2)]
            va = [alloc([32, 2, 128], BF16, name=f"a_v{i}") for i in range(2)]
            biasG = [alloc([32, 2], F32, name=f"a_bias{i}") for i in range(2)]
            NP = 4
            pT = [alloc([GS], BF16, name=f"a_pT{i}") for i in range(NP)]
            sgl = [alloc([GS], F32, name=f"a_sg{i}") for i in range(2)]
            Rr = [alloc([GS], F32, name=f"a_R{i}") for i in range(2)]
            Rg = [alloc([GS], F32, name=f"a_Rg{i}") for i in range(2)]
            stb = [banks[4], banks[5], banks[6]]
            NG = NTOK // GS
            items = []
            for c in range(8):
                for G in range(NG):
                    nj = 4 * G + 4
                    for e in range(2):
                        for j in range(nj):
                            items.append((c, G, e, j, nj))
            n_items = len(items)
            D = 2
            deferred = {}

            def load_pair(c):
                load(qc[c % 2][0][0:64, :], qT[c * 128:c * 128 + 64, :])
                load(qc[c % 2][1][64:128, :], qT[c * 128 + 64:(c + 1) * 128, :])
                load(kc_[c % 2], kT[c * 128:(c + 1) * 128, :])
                for e in range(2):
                    load(va[c % 2][:, :, e, :], vaug[2 * c + e].rearrange("(j p) d -> p j d", p=128))

            def grp(c, G):
                return c * NG + G

            def emitA(k):
                c, G, e, j, nj = items[k]
                it = grp(c, G)
                if e == 0 and j == 0:
                    BG = biasG[it % 2]
                    load(sgl[it % 2], sgT[c * 128:(c + 1) * 128, G * GS:(G + 1) * GS])
                    for e2 in range(2):
                        h = 2 * c + e2
                        ts("vector", BG[:, 0:nj, e2], L_tm[:, 0:nj, h], 1.0, cumtot[:, nj - 1, h:h + 1], ALU.mult, ALU.subtract)
                i = j - 4 * G
                qs = max(0, i) * 128
                N = GS - qs
                st = stb[k % 3]
                if i >= 0:
                    mm(st[:, 0:N], kc_[c % 2][:, j * 128:(j + 1) * 128], qc[c % 2][e][:, G * GS + qs:(G + 1) * GS],
                       start=True, stop=False)
                    mm(st[:, 0:128], ident, negmask, start=False, stop=True)
                else:
                    mm(st[:, 0:N], kc_[c % 2][:, j * 128:(j + 1) * 128], qc[c % 2][e][:, G * GS + qs:(G + 1) * GS])

            def emitBC(k, step):
                c, G, e, j, nj = items[k]
                it = grp(c, G)
                BG = biasG[it % 2]
                O = [banks[(it % 2) * 2], banks[(it % 2) * 2 + 1]]
                i = j - 4 * G
                qs = max(0, i) * 128
                N = GS - qs
                st = stb[k % 3]
                P_ = pT[k % NP]
                act(P_[:, 0:N], st[:, 0:N], AF.Exp, bias=BG[:, j, e:e + 1])
                mm(O[e][:, qs:GS], va[c % 2][:, j, e, :], P_[:, 0:N], start=(j == 0), stop=(j == nj - 1))
                if e == 1 and j == nj - 1:
                    R_ = Rr[it % 2]
                    RG_ = Rg[it % 2]
                    SGL = sgl[it % 2]

                    def fin(c=c, G=G, O=O, R_=R_, RG_=RG_, SGL=SGL):
                        act(R_[0:64, :], O[1][0:64, :], AF.Ln)
                        act(R_[64:128, :], O[0][64:128, :], AF.Ln)
                        act(R_, R_, AF.Exp, scale=-1.0)
                        psw = banks[7]
                        mm(psw, swp, R_)
                        tt("vector", RG_, psw, SGL, ALU.mult)
                        tt("vector", oT[0:64, c, G * GS:(G + 1) * GS], O[0][0:64, :], RG_[0:64, :], ALU.mult)
                        tt("vector", oT[64:128, c, G * GS:(G + 1) * GS], O[1][64:128, :], RG_[64:128, :], ALU.mult)
                    deferred.setdefault(step + 2, []).append(fin)
                    if G == NG - 1 and c + 2 < 8:
                        load_pair(c + 2)

            load_pair(0)
            load_pair(1)
            step = 0
            while step < n_items + D or any(k >= step for k in deferred):
                if step < n_items:
                    emitA(step)
                if 0 <= step - D < n_items:
                    emitBC(step - D, step)
                for f in deferred.pop(step, []):
                    f()
                step += 1
            S.barrier()
            off[0] = p4_mark
            set_banks(range(8))
            w_out = alloc([8, 1024], BF16, name="o_w_out")
            oov = od_w_out_b.rearrange("p (k n) -> p k n", k=8)
            for kc in range(0, 8, 4):
                load(w_out[:, kc:kc + 4, :], oov[:, kc:kc + 4, :])
            rbuf = [alloc([8, GS], F32, name=f"p5_r{i}") for i in range(3)]
            lt = (alloc([8, GS], BF16, name="p5_rb"), alloc([8, GS], BF16, name="p5_sq"),
                  alloc([GS], F32, name="p5_mean"), alloc([GS], F32, name="p5_rstd"))
            tA = [alloc([GS], F32, name=f"p5_tA{i}") for i in range(4)]
            srcv = x2T.rearrange("(kc p) t -> p kc t", p=128)
            dstv = x3T.rearrange("(kc p) t -> p kc t", p=128)
            if "d_oT" in dbg:
                for c in range(8):
                    for G in range(NTOK // GS):
                        cp("vector", rbuf[0][:, 0, :], oT[:, c, G * GS:(G + 1) * GS])
                        store(dbg_aps["d_oT"][c * 128:(c + 1) * 128, G * GS:(G + 1) * GS], rbuf[0][:, 0, :])
            NG = NTOK // GS
            load(rbuf[0], srcv[:, :, 0:GS])
            ui = 0
            prev = None
            for g in range(NG):
                t0 = g * GS
                R = rbuf[g % 3]
                for oc in range(8):
                    a_ = tA[ui % 4]
                    ui += 1
                    ps = pb()
                    for kc in range(8):
                        mm(ps, w_out[:, kc, oc * 128:(oc + 1) * 128], oT[:, kc, t0:t0 + GS], start=(kc == 0), stop=(kc == 7))
                    act(a_, ps, AF.Identity, scale=gate_(2, oc))
                    stt("vector", R[:, oc, :], R[:, oc, :], ALPHA, a_, ALU.mult, ALU.add)
                    if oc == 1 and prev is not None:
                        prev[1]()
                        prev[2]()
                    if oc == 3 and prev is not None:
                        prev[3]()
                    if oc == 5 and prev is not None:
                        prev[4]()
                        if g + 1 < NG:
                            load(rbuf[(g + 1) % 3], srcv[:, :, t0 + GS:t0 + 2 * GS])
                if g == 0 and NG > 1:
                    load(rbuf[1], srcv[:, :, GS:2 * GS])
                stg = ln_stages(R, R, 2, GS, lt, after=(lambda R=R, t0=t0: store(dstv[:, :, t0:t0 + GS], R)))
                stg[0]()
                prev = stg
            prev[1]()
            prev[2]()
            prev[3]()
            prev[4]()

        plan = [
            (1, phase1),
            (2, lambda: phase_mlp(0, x1T, x2T)),
            (3, phase3),
            (4, phase45),
            (5, lambda: phase_mlp(1, x3T, outT)),
        ]
        for pid, fn in plan:
            if pid > last_phase:
                break
            fn()
            S.barrier()
            off[0] = persist_mark
        S.finish()
        stats = {e: len(S.ops[e]) for e in S.ENG}
        stats["signal"] = S.n_signal
    return nc, stats


def _fm(v, nchunk):
    return np.ascontiguousarray(np.asarray(v, np.float32).reshape(nchunk, 128).T)


def _wl(w):
    K, N = w.shape
    return np.ascontiguousarray(np.asarray(w, np.float32).reshape(K // 128, 128, N).transpose(1, 0, 2))


def prepare_inputs(x, c, ada_w, ada_b, ln_g, ln_b, ev_w_in, ev_conv_w, ev_conv_b, ev_rg_wa, ev_rg_ba, ev_rg_wx,
                   ev_rg_bx, ev_rg_lam, ev_gla_w_up, ev_gla_b_up, ev_gla_norm_g, ev_w_out, od_w_in, od_b_f,
                   od_q_norm_g, od_k_norm_g, od_w_out, mlp_w1, mlp_b1, mlp_w2, mlp_b2):
    f = lambda a: np.ascontiguousarray(np.asarray(a, np.float32))
    shared = {
        "ada_w": np.stack([_wl(ada_w[l, j]) for l in range(2) for j in range(2)]),
        "ada_b": np.ascontiguousarray(np.stack([_fm(ada_b[l, j], 24) for l in range(2) for j in range(2)], axis=1)),
        "ln_g": np.ascontiguousarray(np.stack([_fm(ln_g[l, j], 8) for l in range(2) for j in range(2)], axis=1)),
        "ln_b": np.ascontiguousarray(np.stack([_fm(ln_b[l, j], 8) for l in range(2) for j in range(2)], axis=1)),
        "ev_w_in": _wl(ev_w_in[0]),
        "ev_conv_w": np.ascontiguousarray(np.asarray(ev_conv_w[0], np.float32).reshape(4, 4, 128).transpose(2, 1, 0)),
        "ev_conv_b": _fm(ev_conv_b[0], 4),
        "ev_rg_wa": f(ev_rg_wa[0]),
        "ev_rg_ba": _fm(ev_rg_ba[0], 4),
        "ev_rg_wx": f(ev_rg_wx[0]),
        "ev_rg_bx": _fm(ev_rg_bx[0], 4),
        "ev_rg_lam": _fm(ev_rg_lam[0], 4),
        "ev_gla_w_up": f(ev_gla_w_up[0]),
        "ev_gla_b_up": np.ascontiguousarray(np.asarray(ev_gla_b_up[0], np.float32).reshape(4, 64).T),
        "ev_gla_norm_g": _fm(ev_gla_norm_g[0], 4),
        "ev_w_out": _wl(ev_w_out[0]),
        "od_w_in": _wl(od_w_in[0]),
        "od_b_f": f(od_b_f[0]),
        "od_qk_g": np.ascontiguousarray(np.stack([np.tile(np.asarray(od_q_norm_g[0], np.float32), 2),
                                                  np.tile(np.asarray(od_k_norm_g[0], np.float32), 2)], axis=1)),
        "od_w_out": _wl(od_w_out[0]),
        "mlp_w1": np.stack([_wl(mlp_w1[l]) for l in range(2)]),
        "mlp_b1": np.ascontiguousarray(np.stack([_fm(mlp_b1[l], 32) for l in range(2)], axis=1)),
        "mlp_w2": np.stack([_wl(mlp_w2[l]) for l in range(2)]),
        "mlp_b2": np.ascontiguousarray(np.stack([_fm(mlp_b2[l], 8) for l in range(2)], axis=1)),
    }
    x = np.asarray(x, np.float32)
    c = np.asarray(c, np.float32)
    in_maps = []
    for b in range(x.shape[0]):
        m = dict(shared)
        m["xT"] = np.ascontiguousarray(x[b].T)
        m["c_fm"] = _fm(c[b], 8)
        in_maps.append(m)
    return in_maps


_CACHE = {}


def kernel(**inputs):
    in_maps = prepare_inputs(**inputs)
    if "nc" not in _CACHE:
        _CACHE["nc"] = build_program()[0]
    nc = _CACHE["nc"]
    res = run_bass_kernel_spmd(nc, in_maps, core_ids=list(range(len(in_maps))))
    out = np.stack([np.ascontiguousarray(np.asarray(r["outT"], np.float32).T) for r in res.results], axis=0)
    return out
```
